# Optimizing a Trainium2 kernel written in Bass

```python
import jax, jax.numpy as jnp
from jax import lax
import numpy as np

D_MODEL = 1024
BATCH = 4
SEQ = 8192
DEPTH = 1

GLA_HEADS = 4
GLA_DK = 128
GLA_DV = 256
GLA_KEY_W = GLA_HEADS * GLA_DK
GLA_VAL_W = GLA_HEADS * GLA_DV
GLA_GATE_RANK = 16
GLA_TAU = 16.0
GLA_CHUNK = 64
SWA_HEADS = 16
SWA_KV_HEADS = 2
SWA_GROUP = SWA_HEADS // SWA_KV_HEADS
SWA_DH = 64
SWA_W = SWA_HEADS * SWA_DH
SWA_KV_W = SWA_KV_HEADS * SWA_DH
WINDOW = 128
N_GROUPS = 4
EXPERTS_PER_GROUP = 4
N_EXPERTS = N_GROUPS * EXPERTS_PER_GROUP
TOP_K = 2
D_EXPERT = 256
N_MOD = 6
EPS = 1e-6
IN_WIDTHS = (GLA_KEY_W, GLA_KEY_W, GLA_VAL_W, GLA_VAL_W, GLA_GATE_RANK,
             SWA_W, SWA_KV_W, SWA_KV_W, D_MODEL, D_MODEL)
IN_SPLITS = tuple(int(s) for s in np.cumsum(IN_WIDTHS)[:-1])
IN_TOTAL = int(sum(IN_WIDTHS))

kernel_name = "hybrid_gla_swa_sink_hmoe_adaln"


def rms_norm(x, g):
    x32 = x.astype(jnp.float32)
    y = x32 * lax.rsqrt(jnp.mean(x32 * x32, axis=-1, keepdims=True) + EPS)
    return (y * g.astype(jnp.float32)).astype(x.dtype)


def gla_mixer(q, k, v, r, z_gate, w_gk2, b_gk, gla_norm_g):
    B, S, _ = q.shape
    N = S // GLA_CHUNK
    dt = q.dtype
    f32 = jnp.float32
    shp = (B, N, GLA_CHUNK, GLA_HEADS)
    q = q.astype(f32).reshape(*shp, GLA_DK) * (GLA_DK ** -0.5)
    k = k.astype(f32).reshape(*shp, GLA_DK)
    v = v.astype(f32).reshape(*shp, GLA_DV)
    log_a = jax.nn.log_sigmoid((z_gate @ w_gk2 + b_gk).astype(f32)) / GLA_TAU
    log_a = log_a.reshape(*shp, GLA_DK)
    b = jnp.cumsum(log_a, axis=2)
    b_last = b[:, :, -1]
    q_dec = q * jnp.exp(b)
    k_inv = k * jnp.exp(-b)
    causal = jnp.tril(jnp.ones((GLA_CHUNK, GLA_CHUNK), dtype=bool))
    att = jnp.einsum('bnihk,bnjhk->bnhij', q_dec, k_inv)
    att = jnp.where(causal, att, 0.0)
    o_intra = jnp.einsum('bnhij,bnjhv->bnihv', att, v)
    k_to_end = k * jnp.exp(b_last[:, :, None] - b)
    upd = jnp.einsum('bnjhk,bnjhv->bnhkv', k_to_end, v)
    decay = jnp.exp(b_last)

    def step(state, inp):
        dec, u = inp
        return dec[..., None] * state + u, state

    s0 = jnp.zeros((B, GLA_HEADS, GLA_DK, GLA_DV), f32)
    _, s_prev = lax.scan(step, s0, (jnp.moveaxis(decay, 1, 0), jnp.moveaxis(upd, 1, 0)))
    s_prev = jnp.moveaxis(s_prev, 0, 1)
    o_inter = jnp.einsum('bnihk,bnhkv->bnihv', q_dec, s_prev)
    o = (o_intra + o_inter).reshape(B, S, GLA_HEADS, GLA_DV)
    o = o * lax.rsqrt(jnp.mean(o * o, axis=-1, keepdims=True) + EPS) * gla_norm_g.astype(f32)
    o = o.reshape(B, S, GLA_VAL_W) * jax.nn.silu(r.astype(f32))
    return o.astype(dt)


def swa_mixer(q, k, v, sink):
    B, S, _ = q.shape
    nb = S // WINDOW
    dt = q.dtype
    f32 = jnp.float32
    q = q.reshape(B, nb, WINDOW, SWA_KV_HEADS, SWA_GROUP, SWA_DH)
    k = k.reshape(B, nb, WINDOW, SWA_KV_HEADS, SWA_DH)
    v = v.reshape(B, nb, WINDOW, SWA_KV_HEADS, SWA_DH)

    def with_prev(t):
        prev = jnp.concatenate([jnp.zeros_like(t[:, :1]), t[:, :-1]], axis=1)
        return jnp.concatenate([prev, t], axis=2)

    kk, vv = with_prev(k), with_prev(v)
    s = jnp.einsum('bnqhgd,bnkhd->bnhgqk', q, kk).astype(f32) * (SWA_DH ** -0.5)
    qi = jnp.arange(WINDOW)[:, None] + WINDOW
    kj = jnp.arange(2 * WINDOW)[None, :]
    rel = qi - kj
    band = (rel >= 0) & (rel < WINDOW)
    blk = jnp.arange(nb)[:, None, None]
    valid = band[None] & ((blk > 0) | (kj[None] >= WINDOW))
    s = jnp.where(valid[None, :, None, None], s, -jnp.inf)
    sink_logit = jnp.broadcast_to(
        sink.astype(f32).reshape(1, 1, SWA_KV_HEADS, SWA_GROUP, 1, 1), s.shape[:-1] + (1,))
    p = jax.nn.softmax(jnp.concatenate([s, sink_logit], axis=-1), axis=-1)[..., :-1]
    o = jnp.einsum('bnhgqk,bnkhd->bnqhgd', p.astype(dt), vv)
    return o.reshape(B, S, SWA_W)


def mixer(h, w_in, w_gk2, b_gk, gla_norm_g, sink, w_o):
    proj = h @ w_in
    qa, ka, va, ra, za, qb, kb, vb, ga, gb = jnp.split(proj, IN_SPLITS, axis=-1)
    oa = gla_mixer(qa, ka, va, ra, za, w_gk2, b_gk, gla_norm_g)
    ob = swa_mixer(qb, kb, vb, sink)
    merged = jax.nn.sigmoid(ga) * oa + jax.nn.sigmoid(gb) * ob
    return merged @ w_o


def hier_moe(h, w_group, b_group, w_router, b_router, w_gate, w_up, w_down):
    B, S, D = h.shape
    f32 = jnp.float32
    t = h.reshape(B * S, D)
    g_logits = (t @ w_group + b_group).astype(f32)
    g_prob = jax.nn.softmax(g_logits, axis=-1)
    g_idx = jnp.argmax(g_logits, axis=-1)
    g_w = jnp.take_along_axis(g_prob, g_idx[:, None], axis=1)
    e_logits = (t @ w_router + b_router).astype(f32).reshape(-1, N_GROUPS, EXPERTS_PER_GROUP)
    e_in = jnp.take_along_axis(e_logits, g_idx[:, None, None], axis=1)[:, 0]
    top_v, top_i = lax.top_k(e_in, TOP_K)
    top_w = jax.nn.softmax(top_v, axis=-1) * g_w
    e_idx = g_idx[:, None] * EXPERTS_PER_GROUP + top_i
    combine = jnp.einsum('tk,tke->te', top_w, jax.nn.one_hot(e_idx, N_EXPERTS, dtype=f32))
    hid = jax.nn.silu(jnp.einsum('td,edf->tef', t, w_gate)) * jnp.einsum('td,edf->tef', t, w_up)
    hid = hid * combine.astype(hid.dtype)[:, :, None]
    y = jnp.einsum('tef,efd->td', hid, w_down)
    return y.reshape(B, S, D)


def setup_inputs(seed: int = 0) -> dict:
    key = jax.random.key(seed)
    ks = jax.random.split(key, 20)
    f32 = jnp.float32
    L = DEPTH

    def nrm(k, shape, scale):
        return jax.random.normal(k, shape, f32) * scale

    return {
        "x": nrm(ks[0], (BATCH, SEQ, D_MODEL), 1.0),
        "c": nrm(ks[1], (BATCH, D_MODEL), 1.0),
        "w_ada": nrm(ks[2], (L, D_MODEL, N_MOD * D_MODEL), D_MODEL ** -0.5),
        "b_ada": nrm(ks[3], (L, N_MOD * D_MODEL), 0.02),
        "norm1_g": 1.0 + nrm(ks[4], (L, D_MODEL), 0.02),
        "w_in": nrm(ks[5], (L, D_MODEL, IN_TOTAL), D_MODEL ** -0.5),
        "w_gk2": nrm(ks[6], (L, GLA_GATE_RANK, GLA_KEY_W), GLA_GATE_RANK ** -0.5),
        "b_gk": nrm(ks[7], (L, GLA_KEY_W), 0.1),
        "gla_norm_g": 1.0 + nrm(ks[8], (L, GLA_DV), 0.02),
        "sink": nrm(ks[9], (L, SWA_HEADS), 1.0),
        "w_o": nrm(ks[10], (L, D_MODEL, D_MODEL), D_MODEL ** -0.5),
        "norm2_g": 1.0 + nrm(ks[11], (L, D_MODEL), 0.02),
        "w_group": nrm(ks[12], (L, D_MODEL, N_GROUPS), D_MODEL ** -0.5),
        "b_group": nrm(ks[13], (L, N_GROUPS), 0.01),
        "w_router": nrm(ks[14], (L, D_MODEL, N_EXPERTS), D_MODEL ** -0.5),
        "b_router": nrm(ks[15], (L, N_EXPERTS), 0.01),
        "w_gate": nrm(ks[16], (L, N_EXPERTS, D_MODEL, D_EXPERT), D_MODEL ** -0.5),
        "w_up": nrm(ks[17], (L, N_EXPERTS, D_MODEL, D_EXPERT), D_MODEL ** -0.5),
        "w_down": nrm(ks[18], (L, N_EXPERTS, D_EXPERT, D_MODEL), D_EXPERT ** -0.5),
        "norm_f_g": 1.0 + nrm(ks[19], (D_MODEL,), 0.02),
    }


def reference(x, c, w_ada, b_ada, norm1_g, w_in, w_gk2, b_gk, gla_norm_g, sink, w_o, norm2_g,
              w_group, b_group, w_router, b_router, w_gate, w_up, w_down, norm_f_g):
    for l in range(DEPTH):
        mod = jax.nn.silu(c) @ w_ada[l] + b_ada[l]
        sh1, sc1, gt1, sh2, sc2, gt2 = jnp.split(mod[:, None, :], N_MOD, axis=-1)
        h = rms_norm(x, norm1_g[l]) * (1.0 + sc1) + sh1
        x = x + gt1 * mixer(h, w_in[l], w_gk2[l], b_gk[l], gla_norm_g[l], sink[l], w_o[l])
        h = rms_norm(x, norm2_g[l]) * (1.0 + sc2) + sh2
        x = x + gt2 * hier_moe(h, w_group[l], b_group[l], w_router[l], b_router[l],
                               w_gate[l], w_up[l], w_down[l])
    return rms_norm(x, norm_f_g)
```

```python
import numpy as np
from contextlib import ExitStack
import concourse.bass as bass
import concourse.mybir as mybir
from concourse.bass_utils import run_bass_kernel_spmd

F32 = mybir.dt.float32
BF16 = mybir.dt.bfloat16
AF = mybir.ActivationFunctionType
ALU = mybir.AluOpType

EPS = 1e-6
EPS_GLA = 1e-6 * 128.0


class Sched:
    def __init__(self, nc):
        self.nc = nc
        self.eng = {"pe": nc.tensor, "act": nc.scalar, "dve": nc.vector, "pool": nc.gpsimd, "sp": nc.sync}
        self.sem, self.cnt, self.seen, self.pend = {}, {}, {}, {}
        for e in self.eng:
            self.sem[e] = nc.alloc_semaphore("s_" + e)
            self.cnt[e] = 0
            self.seen[e] = {}
            self.pend[e] = []
        self.last_w, self.readers, self.dma_sems = {}, {}, {}

    def _wait(self, e, tok):
        sem, val, src = tok
        if src == e and e == "pe":
            return
        key = id(sem)
        if self.seen[e].get(key, 0) >= val:
            return
        self.seen[e][key] = val
        self.eng[e].wait_ge(sem, val)

    def deps(self, e, reads, writes):
        for r in reads:
            t = self.last_w.get(r)
            if t is not None:
                self._wait(e, t)
        for w in writes:
            t = self.last_w.get(w)
            if t is not None:
                self._wait(e, t)
            for t in self.readers.get(w, ()):
                self._wait(e, t)

    def commit(self, tok, reads, writes):
        for r in reads:
            self.readers.setdefault(r, []).append(tok)
        for w in writes:
            self.last_w[w] = tok
            self.readers[w] = []

    def op(self, e, reads, writes, fn, last=True):
        self.deps(e, reads, writes)
        ins = fn(self.eng[e])
        self.pend[e].append((reads, writes))
        if last:
            self.cnt[e] += 1
            ins.then_inc(self.sem[e], 1)
            tok = (self.sem[e], self.cnt[e], e)
            for (r, w) in self.pend[e]:
                self.commit(tok, r, w)
            self.pend[e] = []

    def dma(self, q, semname, reads, writes, out, in_):
        self.deps(q, reads, writes)
        if semname not in self.dma_sems:
            self.dma_sems[semname] = [self.nc.alloc_semaphore("d_" + semname), 0]
        ds = self.dma_sems[semname]
        ds[1] += 16
        self.eng[q].dma_start(out=out, in_=in_).then_inc(ds[0], 16)
        self.commit((ds[0], ds[1], "dma"), reads, writes)

    def barrier(self):
        toks = [(self.sem[e], self.cnt[e], e) for e in self.eng if self.cnt[e] > 0]
        toks += [(s, v, "dma") for (s, v) in self.dma_sems.values()]
        for e in self.eng:
            for t in toks:
                if t[2] == e:
                    continue
                self._wait(e, t)
        self.last_w.clear()
        self.readers.clear()

    def finish(self, e="sp"):
        for (sem, val) in self.dma_sems.values():
            self._wait(e, (sem, val, "dma"))


def build(nc, NTM, NTP):
    M, P = NTM * 128, NTP * 128
    NGM, NGP = NTM // 4, NTP // 4

    def dr(name, shape, dt=F32, kind="ExternalInput"):
        return nc.dram_tensor(name, shape, dt, kind=kind).ap()

    xm = dr("xm", [M, 1024]); xp = dr("xp", [P, 1024]); flag = dr("flag", [128, 1])
    c_pm = dr("c_pm", [128, 8]); w_ada = dr("w_ada", [1024, 6144]); bada_pm = dr("bada_pm", [128, 48])
    bada_bc = dr("bada_bc", [128, 2, 1024])
    g1_pm = dr("g1_pm", [128, 8]); g2_pm = dr("g2_pm", [128, 8]); gf_bc = dr("gf_bc", [128, 1024])
    w_in = dr("w_in", [1024, 6416]); wgk = dr("wgk", [17, 512]); gng_bc = dr("gng_bc", [128, 1024])
    sink_bc = dr("sink_bc", [128, 16]); w_o = dr("w_o", [1024, 1024])
    w_rt = dr("w_rt", [1024, 20]); b_rt = dr("b_rt", [1, 20])
    w_gate = dr("w_gate", [16, 1024, 256]); w_up = dr("w_up", [16, 1024, 256]); w_down = dr("w_down", [16, 256, 1024])
    out = dr("out", [M, 1024], kind="ExternalOutput")
    oa_s = dr("oa_s", [M, 1024], BF16, kind="Internal")
    x1_s = dr("x1_s", [M, 1024], F32, kind="Internal")
    wgu_s = dr("wgu_s", [16, 128, 8, 512], BF16, kind="Internal")
    wB_s = dr("wB_s", [128, 8, 2304], BF16, kind="Internal")
    wo_s = dr("wo_s", [128, 8, 1024], BF16, kind="Internal")

    S = Sched(nc)
    es0 = ExitStack()

    def sb(es, name, shape, dt=F32):
        return es.enter_context(nc.sbuf_tensor(name, shape, dt))

    def ps(es, name, shape, dt=F32):
        return es.enter_context(nc.psum_tensor(name, shape, dt))

    ident_bf = sb(es0, "ident_bf", [128, 128], BF16)
    ident_f = sb(es0, "ident_f", [128, 128], F32)
    tri = sb(es0, "tri", [128, 128], F32)
    maskg = sb(es0, "maskg", [128, 128], F32)
    mcur = sb(es0, "mcur", [128, 128], BF16)
    mprev = sb(es0, "mprev", [128, 128], BF16)
    c_sb = sb(es0, "c_sb", [128, 8]); scc = sb(es0, "scc", [128, 8], BF16)
    sc_bc = sb(es0, "sc_bc", [128, 8, 128], BF16)
    modT = sb(es0, "modT", [128, 48]); badap = sb(es0, "badap", [128, 48])
    g1s = sb(es0, "g1s", [128, 8]); g2s = sb(es0, "g2s", [128, 8])
    sc1g = sb(es0, "sc1g", [128, 8]); sc2g = sb(es0, "sc2g", [128, 8])
    gtb = sb(es0, "gtb", [128, 2, 1024])
    flag_sb = sb(es0, "flag_sb", [128, 1]); fbias = sb(es0, "fbias", [128, 1])
    nhalf = sb(es0, "nhalf", [128, 4])
    expsink = sb(es0, "expsink", [128, 16])

    def tri_fill(t, val, pattern, cm, cmp):
        tn = "const%d" % id(t)
        S.op("pool", [], [tn], lambda e: e.memset(t[:, :], val))
        S.op("pool", [tn], [tn], lambda e: e.affine_select(
            out=t[:, :], in_=t[:, :], pattern=pattern, compare_op=cmp, fill=0.0, base=0, channel_multiplier=cm))

    tri_fill(ident_bf, 1.0, [[-1, 128]], 1, ALU.is_equal)
    tri_fill(ident_f, 1.0, [[-1, 128]], 1, ALU.is_equal)
    tri_fill(tri, -1.0 / 16.0, [[1, 128]], -1, ALU.is_ge)
    tri_fill(maskg, 1.0, [[1, 128]], -1, ALU.is_ge)
    tri_fill(mcur, 1.0, [[1, 128]], -1, ALU.is_ge)
    tri_fill(mprev, 1.0, [[-1, 128]], 1, ALU.is_gt)
    S.op("pool", [], ["nhalf"], lambda e: e.memset(nhalf[:, :], -0.5))
    mbc = sb(es0, "mbc", [128, 4, 128], BF16)
    mbp = sb(es0, "mbp", [128, 4, 128], BF16)
    S.op("pool", [], ["mbc"], lambda e: e.memset(mbc[:, :, :], 0.0))
    S.op("pool", ["mbc"], ["mbc"], lambda e: e.affine_select(
        out=mbc[:, :, :], in_=mbc[:, :, :], pattern=[[0, 4], [1, 128]], compare_op=ALU.is_ge, fill=-30000.0,
        base=0, channel_multiplier=-1))
    S.op("pool", [], ["mbp"], lambda e: e.memset(mbp[:, :, :], 0.0))
    S.op("pool", ["mbp"], ["mbp"], lambda e: e.affine_select(
        out=mbp[:, :, :], in_=mbp[:, :, :], pattern=[[0, 4], [-1, 128]], compare_op=ALU.is_gt, fill=-30000.0,
        base=0, channel_multiplier=1))

    S.dma("sp", "c0", [], ["c_sb"], c_sb[:, :], c_pm[:, :])
    S.dma("sp", "c1", [], ["badap"], badap[:, :], bada_pm[:, :])
    S.dma("sp", "c2", [], ["g1s"], g1s[:, :], g1_pm[:, :])
    S.dma("sp", "c3", [], ["g2s"], g2s[:, :], g2_pm[:, :])
    S.dma("sp", "c4", [], ["gtb"], gtb[:, :, :], bada_bc[:, :, :])
    S.dma("sp", "c5", [], ["flag_sb"], flag_sb[:, :], flag[:, :])
    S.dma("sp", "c6", [], ["expsink"], expsink[:, :], sink_bc[:, :])
    S.op("act", ["expsink"], ["expsink"], lambda e: e.activation(expsink[:, :], expsink[:, :], AF.Exp))
    S.op("act", ["c_sb"], ["scc"], lambda e: e.activation(scc[:, :], c_sb[:, :], AF.Silu))
    S.op("dve", ["scc"], ["sc_bc"], lambda e: e.tensor_copy(
        sc_bc[:, :, :], scc[:, :].unsqueeze(2).to_broadcast([128, 8, 128])))
    S.op("dve", ["flag_sb"], ["fbias"], lambda e: e.tensor_scalar(
        out=fbias[:, :], in0=flag_sb[:, :], scalar1=-1.0, scalar2=30000.0, op0=ALU.add, op1=ALU.mult))

    es1 = ExitStack()
    wA = sb(es1, "wA", [128, 8, 4112], BF16)
    OQA, OKA, OVA, ORA, OZA, OGA = 0, 512, 1024, 2048, 3072, 3088
    wa_blk = [sb(es1, "wa_blk%d" % i, [128, 8, 512], BF16) for i in range(2)]
    wgk_sb = sb(es1, "wgk_sb", [17, 512], BF16)
    gng = sb(es1, "gng", [128, 1024])
    p_tr = ps(es1, "p_tr", [128, 8, 128], BF16)
    p_proj = [ps(es1, "p_proj%d" % i, [128, 512]) for i in range(2)]
    p_b = ps(es1, "p_b", [128, 4, 128])
    p_mod = p_b[:, 0, 0:48]
    p_att = ps(es1, "p_att", [128, 4, 128])
    p_o = ps(es1, "p_o", [128, 1024])
    p_T = ps(es1, "p_T", [128, 2, 256])

    w_in_r = w_in.rearrange("(c p) n -> p c n", p=128)
    w_ada_r = w_ada.rearrange("(c p) n -> p c n", p=128)

    def mod_dma(blk):
        b = blk % 2
        S.dma("pool", "wa%d" % b, [], [("wa", b)], wa_blk[b][:, :, :], w_ada_r[:, :, blk * 512:(blk + 1) * 512])

    def mod_compute(blk):
        b = blk % 2
        v = blk // 2
        if v in (2, 5):
            vi = 0 if v == 2 else 1
            hf = blk % 2
            pp = p_proj[hf]
            for c in range(8):
                S.op("pe", ["sc_bc", ("wa", b)], [("pp", hf)], lambda e: e.matmul(
                    pp[:, :], sc_bc[:, c, :], wa_blk[b][:, c, :], start=(c == 0), stop=(c == 7)), last=(c == 7))
            sl = gtb[:, vi, hf * 512:(hf + 1) * 512]
            S.op("dve", [("pp", hf), "gtb"], ["gtb"], lambda e: e.tensor_tensor(out=sl, in0=pp[:, :], in1=sl, op=ALU.add))
            S.op("dve", ["gtb"], ["gtb"], lambda e: e.tensor_scalar(
                out=sl, in0=sl, scalar1=0.5, scalar2=None, op0=ALU.mult))
        else:
            for q in range(4):
                j = blk * 4 + q
                for c in range(8):
                    S.op("pe", ["scc", ("wa", b)], ["p_b"], lambda e: e.matmul(
                        p_mod[:, j:j + 1], wa_blk[b][:, c, q * 128:(q + 1) * 128], scc[:, c:c + 1],
                        start=(c == 0), stop=(c == 7)), last=(c == 7))
            j0 = blk * 4
            S.op("dve", ["p_b", "badap"], ["modT"], lambda e: e.tensor_tensor(
                out=modT[:, j0:j0 + 4], in0=p_mod[:, j0:j0 + 4], in1=badap[:, j0:j0 + 4], op=ALU.add))

    mod_dma(0)
    mod_dma(1)
    for blk in range(4):
        mod_compute(blk)
        if blk + 2 < 4:
            mod_dma(blk + 2)
    S.op("dve", ["modT", "g1s"], ["sc1g"], lambda e: e.scalar_tensor_tensor(
        out=sc1g[:, :], in0=modT[:, 8:16], scalar=1.0, in1=g1s[:, :], op0=ALU.add, op1=ALU.mult))
    mod_next = [4]

    def mod_deferred():
        blk = mod_next[0]
        if blk >= 12:
            return
        mod_next[0] += 1
        mod_compute(blk)
        if blk + 2 < 12:
            mod_dma(blk + 2)
        if blk == 11:
            S.op("dve", ["modT", "g2s"], ["sc2g"], lambda e: e.scalar_tensor_tensor(
                out=sc2g[:, :], in0=modT[:, 32:40], scalar=1.0, in1=g2s[:, :], op0=ALU.add, op1=ALU.mult))

    def normT(es_bufs, src_rows, hT_ap_fn, scg, sh_off, tag, k):
        xt, junk, ss, rstd, xn, ptr = es_bufs
        b = k % 2
        b3 = k % len(xt)
        S.dma("sp", tag + "x%d" % b3, [], [(tag + "xt", b3)], xt[b3][:, :], src_rows)
        S.op("act", [(tag + "xt", b3)], [tag + "junk", (tag + "ss", b)], lambda e: e.activation(
            junk[:, :], xt[b3][:, :], AF.Square, accum_out=ss[b][:, :]))
        S.op("dve", [(tag + "ss", b)], [(tag + "ss", b)], lambda e: e.tensor_scalar(
            out=ss[b][:, :], in0=ss[b][:, :], scalar1=1.0 / 1024.0, scalar2=EPS, op0=ALU.mult, op1=ALU.add))
        S.op("pool", [(tag + "ss", b), "nhalf"], [(tag + "rstd", b)], lambda e: e.tensor_tensor(
            out=rstd[b][:, :], in0=ss[b][:, :], in1=nhalf[:, 0:1], op=ALU.pow))
        S.op("dve", [(tag + "xt", b3), (tag + "rstd", b)], [(tag + "xn", b)], lambda e: e.tensor_scalar(
            out=xn[b][:, :], in0=xt[b3][:, :], scalar1=rstd[b][:, 0:1], scalar2=None, op0=ALU.mult))
        for c in range(8):
            S.op("pe", [(tag + "xn", b), "ident_bf"], ["p_tr"], lambda e: e.transpose(
                ptr[:, c, :], xn[b][:, c * 128:(c + 1) * 128], ident_bf[:, :]), last=(c == 7))
        return ptr

    def mod_evac(ptr, dst_fn, dst_res, scg, sh_off):
        for c in range(8):
            S.op("dve", ["p_tr", "modT", "sc1g", "sc2g"], [dst_res], lambda e: e.tensor_scalar(
                out=dst_fn(c), in0=ptr[:, c, :], scalar1=scg[:, c:c + 1], scalar2=modT[:, sh_off + c:sh_off + c + 1],
                op0=ALU.mult, op1=ALU.add))

    xt = [sb(es1, "xt%d" % i, [128, 1024]) for i in range(2)]
    junk = sb(es1, "junk", [128, 1024], BF16)
    ss = [sb(es1, "ss%d" % i, [128, 1]) for i in range(2)]
    rstd = [sb(es1, "rstd%d" % i, [128, 1]) for i in range(2)]
    xn = [sb(es1, "xn%d" % i, [128, 1024], BF16) for i in range(2)]
    hT = [sb(es1, "hT%d" % i, [128, 8, 512], BF16) for i in range(2)]
    qaT = sb(es1, "qaT", [128, 4, 512], BF16)
    kaT = sb(es1, "kaT", [128, 4, 512], BF16)
    zT = [sb(es1, "zT%d" % i, [17, 512], BF16) for i in range(2)]
    e_sb = sb(es1, "e_sb", [128, 4, 512])
    l_sb = sb(es1, "l_sb", [128, 4, 512])
    va_sb = [sb(es1, "va_sb%d" % i, [128, 1024], BF16) for i in range(2)]
    thr = sb(es1, "thr", [128, 1024], BF16)
    t1 = sb(es1, "t1", [128, 1024], BF16)
    E_sb = sb(es1, "E_sb", [128, 4, 128]); Einv_sb = sb(es1, "Einv_sb", [128, 4, 128])
    qdecT = sb(es1, "qdecT", [128, 4, 128], BF16); kinvT = sb(es1, "kinvT", [128, 4, 128], BF16)
    kinv_tok = sb(es1, "kinv_tok", [128, 4, 128], BF16); attT_sb = sb(es1, "attT_sb", [128, 4, 128], BF16)
    St = sb(es1, "St", [128, 4, 256]); S_bf = sb(es1, "S_bf", [128, 4, 256], BF16)
    tmpS = sb(es1, "tmpS", [128, 4, 256])
    hTtmp = sb(es1, "hTtmp", [128, 8, 128])
    ssq = sb(es1, "ssq", [128, 4]); rso = sb(es1, "rso", [128, 4])
    otmp = sb(es1, "otmp", [128, 1024])
    oag = [sb(es1, "oag%d" % i, [128, 1024], BF16) for i in range(2)]

    for i in range(2):
        S.op("pool", [], [("zT", i)], lambda e: e.memset(zT[i][:, :], 1.0))
    S.op("pool", [], ["St"], lambda e: e.memset(St[:, :, :], 0.0))
    S.op("pool", [], ["S_bf"], lambda e: e.memset(S_bf[:, :, :], 0.0))

    bufs1 = (xt, junk, ss, rstd, xn, p_tr)
    kctr = [0]

    def A1(src, g, gi):
        for t in range(4):
            r0 = g * 512 + t * 128
            ptr = normT(bufs1, src[r0:r0 + 128, :], None, sc1g, 0, "a", kctr[0])
            kctr[0] += 1
            mod_evac(ptr, lambda c: hT[gi % 2][:, c, t * 128:(t + 1) * 128], ("hT", gi % 2), sc1g, 0)

    evq = [0]

    def evac_copy(dst, src, reads, writes):
        evq[0] += 1
        if evq[0] % 2 == 0:
            S.op("act", reads, writes, lambda e: e.activation(dst, src, AF.Copy))
        else:
            S.op("dve", reads, writes, lambda e: e.tensor_copy(dst, src))

    ppq = [0]

    def fm_proj(w_t, wres, col0, ncol, hbuf, hres, ntok, dst, dres, tok0=0):
        i = ppq[0] % 2
        ppq[0] += 1
        pp = p_proj[i]
        for c in range(8):
            S.op("pe", [wres, hres], [("pp", i)], lambda e: e.matmul(
                pp[0:ncol, 0:ntok], w_t[:, c, col0:col0 + ncol], hbuf[:, c, tok0:tok0 + ntok],
                start=(c == 0), stop=(c == 7)), last=(c == 7))
        evac_copy(dst, pp[0:ncol, 0:ntok], [("pp", i)], [dres])

    def tm_proj(w_t, wres, col0, ncol, hbuf, hres, t):
        i = ppq[0] % 2
        ppq[0] += 1
        pp = p_proj[i]
        for c in range(8):
            S.op("pe", [wres, hres], [("pp", i)], lambda e: e.matmul(
                pp[:, 0:ncol], hbuf[:, c, t * 128:(t + 1) * 128], w_t[:, c, col0:col0 + ncol],
                start=(c == 0), stop=(c == 7)), last=(c == 7))
        return pp, ("pp", i)

    def B1(gi, is_main):
        hb, hr = hT[gi % 2], ("hT", gi % 2)
        for h in range(4):
            fm_proj(wA, "wA_k", OKA + h * 128, 128, hb, hr, 512, kaT[:, h, :], "kaT")
        if is_main:
            for h in range(4):
                fm_proj(wA, "wA_q", OQA + h * 128, 128, hb, hr, 512, qaT[:, h, :], "qaT")
        fm_proj(wA, "wA_k", OZA, 16, hb, hr, 512, zT[gi % 2][0:16, :], ("zT", gi % 2))
        for t in range(4):
            i = ppq[0] % 2
            ppq[0] += 1
            pp = p_proj[i]
            S.op("pe", [("zT", gi % 2), "wgk_sb"], [("pp", i)], lambda e: e.matmul(
                pp[:, :], zT[gi % 2][:, t * 128:(t + 1) * 128], wgk_sb[:, :], start=True, stop=True))
            S.op("act", [("pp", i)], [("e_sb", t)], lambda e: e.activation(e_sb[:, t, :], pp[:, :], AF.Exp, scale=-1.0))
        for t in range(4):
            S.op("act", [("e_sb", t)], [("l_sb", t)], lambda e: e.activation(l_sb[:, t, :], e_sb[:, t, :], AF.Ln, bias=1.0))

    def xload(bufs, src_rows, tag, k):
        b3 = k % len(bufs[0])
        S.dma("sp", tag + "x%d" % b3, [], [(tag + "xt", b3)], bufs[0][b3][:, :], src_rows)

    def norm_n1(bufs, tag, k):
        xt_, junk_, ss_, rstd_, xn_, ptr_ = bufs
        b = k % 2
        b3 = k % len(xt_)
        S.op("act", [(tag + "xt", b3)], [tag + "junk", (tag + "ss", b)], lambda e: e.activation(
            junk_[:, :], xt_[b3][:, :], AF.Square, accum_out=ss_[b][:, :]))

    def norm_n2(bufs, tag, k):
        xt_, junk_, ss_, rstd_, xn_, ptr_ = bufs
        b = k % 2
        S.op("dve", [(tag + "ss", b)], [(tag + "ss", b)], lambda e: e.tensor_scalar(
            out=ss_[b][:, :], in0=ss_[b][:, :], scalar1=1.0 / 1024.0, scalar2=EPS, op0=ALU.mult, op1=ALU.add))
        S.op("pool", [(tag + "ss", b), "nhalf"], [(tag + "rstd", b)], lambda e: e.tensor_tensor(
            out=rstd_[b][:, :], in0=ss_[b][:, :], in1=nhalf[:, 0:1], op=ALU.pow))

    def norm_n3(bufs, tag, k):
        xt_, junk_, ss_, rstd_, xn_, ptr_ = bufs
        b = k % 2
        b3 = k % len(xt_)
        S.op("dve", [(tag + "xt", b3), (tag + "rstd", b)], [(tag + "xn", b)], lambda e: e.tensor_scalar(
            out=xn_[b][:, :], in0=xt_[b3][:, :], scalar1=rstd_[b][:, 0:1], scalar2=None, op0=ALU.mult))

    def normT_pre(bufs, tag, k):
        norm_n1(bufs, tag, k)
        norm_n2(bufs, tag, k)
        norm_n3(bufs, tag, k)

    def norm_tr(bufs, tag, k):
        xt_, junk_, ss_, rstd_, xn_, ptr_ = bufs
        b = k % 2
        for c in range(8):
            S.op("pe", [(tag + "xn", b), "ident_bf"], ["p_tr"], lambda e: e.transpose(
                ptr_[:, c, :], xn_[b][:, c * 128:(c + 1) * 128], ident_bf[:, :]), last=(c == 7))

    def norm_ev(bufs, dst3, dst_res, tmp3, tmp_res="hTtmp"):
        ptr_ = bufs[5]
        S.op("dve", ["p_tr", "sc1g"], [tmp_res], lambda e: e.tensor_tensor(
            out=tmp3[:, :, :], in0=ptr_[:, :, :], in1=sc1g[:, :].unsqueeze(2).to_broadcast([128, 8, 128]), op=ALU.mult))
        S.op("dve", [tmp_res, "modT"], [dst_res], lambda e: e.tensor_tensor(
            out=dst3, in0=tmp3[:, :, :], in1=modT[:, 0:8].unsqueeze(2).to_broadcast([128, 8, 128]), op=ALU.add))

    def normT_pe(bufs, tag, k, dst_fn, dst_res):
        xt_, junk_, ss_, rstd_, xn_, ptr_ = bufs
        b = k % 2
        for c in range(8):
            S.op("pe", [(tag + "xn", b), "ident_bf"], ["p_tr"], lambda e: e.transpose(
                ptr_[:, c, :], xn_[b][:, c * 128:(c + 1) * 128], ident_bf[:, :]), last=(c == 7))
        mod_evac(ptr_, dst_fn, dst_res, sc1g, 0)

    t1b = [t1, sb(es1, "t1b", [128, 1024], BF16)]

    def P_va(gi, t, kt):
        hb, hr = hT[gi % 2], ("hT", gi % 2)
        va = va_sb[kt % 2]
        for hf in range(2):
            pp, pr = tm_proj(wA, "wA_k", OVA + hf * 512, 512, hb, hr, t)
            evac_copy(va[:, hf * 512:(hf + 1) * 512], pp[:, :], [pr], [("va", kt % 2)])

    def P_ra(gi, t, kt):
        hb, hr = hT[gi % 2], ("hT", gi % 2)
        tt = t1b[kt % 2]
        for hf in range(2):
            sl = slice(hf * 512, (hf + 1) * 512)
            pp, pr = tm_proj(wA, "wA_q", ORA + hf * 512, 512, hb, hr, t)
            S.op("act", [pr], ["thr"], lambda e: e.activation(thr[:, sl], pp[:, :], AF.Tanh, scale=0.5))
            S.op("dve", [pr, "thr"], [("t1", kt % 2)], lambda e: e.scalar_tensor_tensor(
                out=tt[:, sl], in0=thr[:, sl], scalar=1.0, in1=pp[:, :], op0=ALU.add, op1=ALU.mult))

    def P_ga(gi, t, kt):
        hb, hr = hT[gi % 2], ("hT", gi % 2)
        tt = t1b[kt % 2]
        for hf in range(2):
            sl = slice(hf * 512, (hf + 1) * 512)
            pp, pr = tm_proj(wA, "wA_q", OGA + hf * 512, 512, hb, hr, t)
            S.op("act", [pr], ["thr"], lambda e: e.activation(thr[:, sl], pp[:, :], AF.Tanh, scale=0.5))
            S.op("dve", ["thr", ("t1", kt % 2)], [("t1", kt % 2)], lambda e: e.scalar_tensor_tensor(
                out=tt[:, sl], in0=thr[:, sl], scalar=1.0, in1=tt[:, sl], op0=ALU.add, op1=ALU.mult))

    E2 = [E_sb, sb(es1, "E_sb1", [128, 4, 128])]
    Ei2 = [Einv_sb, sb(es1, "Einv_sb1", [128, 4, 128])]
    kT2 = [kinvT, sb(es1, "kinvT1", [128, 4, 128], BF16)]
    kk2 = [kinv_tok, sb(es1, "kinv_tok1", [128, 4, 128], BF16)]
    pb2 = [p_b, p_att]
    pbn = ["p_b", "p_att"]

    def G_a1(t, par=0):
        for h in range(4):
            S.op("pe", [("l_sb", t), "tri"], [pbn[par]], lambda e: e.matmul(
                pb2[par][:, h, :], l_sb[:, t, h * 128:(h + 1) * 128], tri[:, :], start=True, stop=True), last=(h == 3))

    def G_a2(par=0):
        S.op("act", [pbn[par]], [("E_sb", par)], lambda e: e.activation(E2[par][:, :, :], pb2[par][:, :, :], AF.Exp))
        S.op("act", [pbn[par]], [("Einv_sb", par)], lambda e: e.activation(
            Ei2[par][:, :, :], pb2[par][:, :, :], AF.Exp, scale=-1.0))

    def G_a3(t, is_main, par=0):
        tsl = slice(t * 128, (t + 1) * 128)
        S.op("dve", ["kaT", ("Einv_sb", par)], [("kinvT", par)], lambda e: e.tensor_tensor(
            out=kT2[par][:, :, :], in0=kaT[:, :, tsl], in1=Ei2[par][:, :, :], op=ALU.mult))
        if is_main:
            S.op("dve", ["qaT", ("E_sb", par)], ["qdecT"], lambda e: e.tensor_tensor(
                out=qdecT[:, :, :], in0=qaT[:, :, tsl], in1=E2[par][:, :, :], op=ALU.mult))

    def G_b1a(par=0):
        for h in range(4):
            S.op("pe", [("kinvT", par), "ident_bf"], ["p_tr"], lambda e: e.transpose(
                p_tr[:, h, :], kT2[par][:, h, :], ident_bf[:, :]), last=(h == 3))
        S.op("act", ["p_tr"], [("kinv_tok", par)], lambda e: e.activation(kk2[par][:, :, :], p_tr[:, 0:4, :], AF.Copy))

    def G_b1b(is_main):
        if is_main:
            for h in range(4):
                S.op("pe", [("kinvT", 0), "qdecT"], ["p_att"], lambda e: e.matmul(
                    p_att[:, h, :], kinvT[:, h, :], qdecT[:, h, :], start=True, stop=True), last=(h == 3))
            S.op("dve", ["p_att", "maskg"], ["attT_sb"], lambda e: e.tensor_tensor(
                out=attT_sb[:, :, :], in0=p_att[:, :, :], in1=maskg[:, :].unsqueeze(1).to_broadcast([128, 4, 128]),
                op=ALU.mult))

    p_T2 = [pb2[i][:, :, :].rearrange("p h q -> p (h q)").rearrange("p (j v) -> p j v", j=2) for i in range(2)]

    def G_T(kt, is_main, par=0, tb=None):
        va = va_sb[kt % 2]
        tb = par if tb is None else tb
        for h in range(4):
            dst = p_T[:, h, :] if h < 2 else p_T2[tb][:, h - 2, :]
            S.op("pe", [("kinv_tok", par), ("va", kt % 2)], ["p_T" if h < 2 else pbn[tb]], lambda e: e.matmul(
                dst, kk2[par][:, h, :], va[:, h * 256:(h + 1) * 256], start=True, stop=True), last=(h % 2 == 1))
        S.op("dve", ["p_T", "St"], ["tmpS"], lambda e: e.tensor_tensor(
            out=tmpS[:, 0:2, :], in0=p_T[:, :, :], in1=St[:, 0:2, :], op=ALU.add))
        S.op("dve", [pbn[tb], "St"], ["tmpS"], lambda e: e.tensor_tensor(
            out=tmpS[:, 2:4, :], in0=p_T2[tb], in1=St[:, 2:4, :], op=ALU.add))
        S.op("dve", ["tmpS", ("E_sb", par)], ["St"], lambda e: e.tensor_tensor(
            out=St[:, :, :], in0=tmpS[:, :, :], in1=E2[par][:, :, 127:128].to_broadcast([128, 4, 256]), op=ALU.mult))
        if is_main:
            for h in range(4):
                S.op("act", ["tmpS", ("E_sb", par)], ["S_bf"], lambda e: e.activation(
                    S_bf[:, h, :], tmpS[:, h, :], AF.Identity, scale=E2[par][:, h, 127:128]))

    def G_o(kt, row0):
        va = va_sb[kt % 2]
        tt = t1b[kt % 2]
        for h in range(4):
            S.op("pe", ["attT_sb", ("va", kt % 2)], ["p_o"], lambda e: e.matmul(
                p_o[:, h * 256:(h + 1) * 256], attT_sb[:, h, :], va[:, h * 256:(h + 1) * 256],
                start=True, stop=False), last=False)
            S.op("pe", ["qdecT", "S_bf"], ["p_o"], lambda e: e.matmul(
                p_o[:, h * 256:(h + 1) * 256], qdecT[:, h, :], S_bf[:, h, :],
                start=False, stop=True), last=(h == 3))

    def G_S():
        S.op("pool", ["St"], ["S_bf"], lambda e: e.tensor_copy(S_bf[:, :, :], St[:, :, :]))

    def G_n1(kt):
        for h in range(4):
            S.op("act", ["p_o"], ["otmp", "ssq"], lambda e: e.activation(
                otmp[:, h * 256:(h + 1) * 256], p_o[:, h * 256:(h + 1) * 256], AF.Square, accum_out=ssq[:, h:h + 1]))
        S.op("dve", ["ssq"], ["ssq"], lambda e: e.tensor_scalar(
            out=ssq[:, :], in0=ssq[:, :], scalar1=4.0 / 256.0, scalar2=4.0 * EPS_GLA, op0=ALU.mult, op1=ALU.add))
        S.op("pool", ["ssq", "nhalf"], ["rso"], lambda e: e.tensor_tensor(
            out=rso[:, :], in0=ssq[:, :], in1=nhalf[:, :], op=ALU.pow))

    def G_n2(kt, row0):
        tt = t1b[kt % 2]
        ob = oag[kt % 2]
        for h in range(4):
            sl = slice(h * 256, (h + 1) * 256)
            S.op("dve", ["p_o", "rso", ("t1", kt % 2)], ["otmp"], lambda e: e.scalar_tensor_tensor(
                out=otmp[:, sl], in0=p_o[:, sl], scalar=rso[:, h:h + 1], in1=tt[:, sl], op0=ALU.mult, op1=ALU.mult))
        S.op("pool", ["otmp", "gng"], [("oag", kt % 2)], lambda e: e.tensor_tensor(
            out=ob[:, :], in0=otmp[:, :], in1=gng[:, :], op=ALU.mult))
        S.dma("sp", "oag%d" % (kt % 2), [("oag", kt % 2)], [], oa_s[row0:row0 + 128, :], ob[:, :])

    groups = [(xp, g, False) for g in range(NGP)] + [(xm, g, True) for g in range(NGM)]
    A1(groups[0][0], groups[0][1], 0)
    for c in range(8):
        S.dma("pool", "wAk", [], ["wA_k"], wA[:, c, 512:2048], w_in_r[:, c, 512:2048])
        S.dma("pool", "wAk", [], ["wA_k"], wA[:, c, 3072:3088], w_in_r[:, c, 3072:3088])
    S.dma("pool", "wgk", [], ["wgk_sb"], wgk_sb[:, :], wgk[:, :])
    S.dma("sp", "gng", [], ["gng"], gng[:, :], gng_bc[:, :])
    mod_dma(4)
    mod_dma(5)

    cast_jobs = []
    for c in range(8):
        cast_jobs.append(("wA_q", wA[:, c, 0:512], w_in_r[:, c, 0:512]))
        cast_jobs.append(("wA_q", wA[:, c, 2048:3072], w_in_r[:, c, 2048:3072]))
        cast_jobs.append(("wA_q", wA[:, c, 3088:4112], w_in_r[:, c, 4368:5392]))
    for e_ in range(16):
        cast_jobs.append((("wgu_s", e_), wgu_s[e_][:, :, 0:256], w_gate[e_].rearrange("(c p) f -> p c f", p=128)))
        cast_jobs.append((("wgu_s", e_), wgu_s[e_][:, :, 256:512], w_up[e_].rearrange("(c p) f -> p c f", p=128)))
    for c in range(8):
        for g_ in range(2):
            cast_jobs.append(("wB_s", wB_s[:, c, 0:1024].rearrange("p (m g d) -> p g m d", m=8, g=2)[:, g_],
                              w_in_r[:, c, 3088 + g_ * 512:3600 + g_ * 512].rearrange("p (m d) -> p m d", m=8)))
        cast_jobs.append(("wB_s", wB_s[:, c, 1024:1280], w_in_r[:, c, 4112:4368]))
        cast_jobs.append(("wB_s", wB_s[:, c, 1280:2304], w_in_r[:, c, 5392:6416]))
    cast_jobs.append(("wo_s", wo_s[:, :, :], w_o.rearrange("(c p) n -> p c n", p=128)))
    nslots = [len(groups) * 4]

    def cast_some():
        n = (len(cast_jobs) + nslots[0] - 1) // max(nslots[0], 1)
        nslots[0] -= 1
        for _ in range(n):
            if cast_jobs:
                res_, dst_, src_ = cast_jobs.pop(0)
                S.dma("pool", "wAq" if res_ == "wA_q" else "cast", [], [res_], dst_, src_)

    pend_n = [None]
    nxt_tiles = [(groups[gi_][0], groups[gi_][1] * 512 + t_ * 128) for gi_ in range(1, len(groups)) for t_ in range(4)]
    if nxt_tiles:
        xload(bufs1, nxt_tiles[0][0][nxt_tiles[0][1]:nxt_tiles[0][1] + 128, :], "a", kctr[0])
    for gi, (src, g, is_main) in enumerate(groups):
        if gi == NGP and NGP > 0:
            S.op("dve", ["St", "flag_sb"], ["St"], lambda e: e.tensor_scalar(
                out=St[:, :, :], in0=St[:, :, :], scalar1=flag_sb[:, 0:1], scalar2=None, op0=ALU.mult))
            S.op("pool", ["St"], ["S_bf"], lambda e: e.tensor_copy(S_bf[:, :, :], St[:, :, :]))
        B1(gi, is_main)
        kt0 = gi * 4
        P_va(gi, 0, kt0)
        if is_main:
            P_ra(gi, 0, kt0)
            P_ga(gi, 0, kt0)
        has_next = gi + 1 < len(groups)
        for t in range(4):
            kt = kt0 + t
            row0 = g * 512 + t * 128
            cast_some()
            mod_deferred()
            ka_ = None
            if has_next:
                ka_ = kctr[0]
                kctr[0] += 1
                j_ = gi * 4 + t
                if j_ + 1 < len(nxt_tiles):
                    xload(bufs1, nxt_tiles[j_ + 1][0][nxt_tiles[j_ + 1][1]:nxt_tiles[j_ + 1][1] + 128, :], "a", ka_ + 1)
            if is_main:
                G_a1(t)
                G_a2()
                G_a3(t, is_main)
                if ka_ is not None:
                    norm_n1(bufs1, "a", ka_)
                if pend_n[0] is not None:
                    G_n1(pend_n[0][0])
                if ka_ is not None:
                    norm_n2(bufs1, "a", ka_)
                if t < 3:
                    P_va(gi, t + 1, kt + 1)
                if ka_ is not None:
                    norm_n3(bufs1, "a", ka_)
                G_b1a()
                if pend_n[0] is not None:
                    G_n2(*pend_n[0])
                    pend_n[0] = None
                if t < 3:
                    P_ra(gi, t + 1, kt + 1)
                G_b1b(is_main)
                if ka_ is not None:
                    norm_tr(bufs1, "a", ka_)
                    norm_ev(bufs1, hT[(gi + 1) % 2][:, :, t * 128:(t + 1) * 128], ("hT", (gi + 1) % 2), hTtmp)
                if t < 3:
                    P_ga(gi, t + 1, kt + 1)
                G_o(kt, row0)
                pend_n[0] = (kt, row0)
                G_T(kt, is_main, 0, 1)
            else:
                par = t % 2
                if t == 0:
                    G_a1(t, par)
                    G_a2(par)
                    G_a3(t, False, par)
                    G_b1a(par)
                if ka_ is not None:
                    norm_n1(bufs1, "a", ka_)
                if t < 3:
                    P_va(gi, t + 1, kt + 1)
                    G_a1(t + 1, 1 - par)
                if ka_ is not None:
                    norm_n2(bufs1, "a", ka_)
                if t < 3:
                    G_a2(1 - par)
                if ka_ is not None:
                    norm_n3(bufs1, "a", ka_)
                G_T(kt, False, par)
                if t < 3:
                    G_a3(t + 1, False, 1 - par)
                if ka_ is not None:
                    norm_tr(bufs1, "a", ka_)
                    norm_ev(bufs1, hT[(gi + 1) % 2][:, :, t * 128:(t + 1) * 128], ("hT", (gi + 1) % 2), hTtmp)
                if t < 3:
                    G_b1a(1 - par)
    if pend_n[0] is not None:
        G_n1(pend_n[0][0])
        G_n2(*pend_n[0])
    while mod_next[0] < 12:
        mod_deferred()

    S.barrier()
    es1.close()

    es_w = ExitStack()
    wd = sb(es_w, "wd", [128, 16, 2, 1024], BF16)
    es2 = ExitStack()
    wB = sb(es2, "wB", [128, 8, 2304], BF16)
    OQB, OKB, OVB, OGB = 0, 1024, 1152, 1280
    wo = sb(es2, "wo", [128, 8, 1024], BF16)
    for c in range(8):
        S.dma("sp", "wB", ["wB_s"], ["wB"], wB[:, c, :], wB_s[:, c, :])
    S.dma("sp", "wo", ["wo_s"], ["wo"], wo[:, :, :], wo_s[:, :, :])
    wd_jobs = list(range(16))
    wd_slots = [min(NTM, 16)]

    def wd_some():
        if wd_slots[0] <= 0:
            return
        n = (len(wd_jobs) + wd_slots[0] - 1) // wd_slots[0]
        wd_slots[0] -= 1
        for _ in range(n):
            if wd_jobs:
                e_ = wd_jobs.pop(0)
                S.dma("pool", "wd", [], ["wd"], wd[:, e_, :, :], w_down[e_].rearrange("(fc p) n -> p fc n", p=128))
    p_tr = ps(es2, "p_trb", [128, 8, 128], BF16)
    p_proj = [ps(es2, "p_projb%d" % i, [128, 512]) for i in range(2)]
    p_sc = [[ps(es2, "p_sc%d%d" % (i, j), [128, 512]) for j in range(2)] for i in range(2)]
    p_ob = ps(es2, "p_ob", [128, 4, 65])
    xt = [sb(es2, "xtb%d" % i, [128, 1024]) for i in range(2)]
    junk = sb(es2, "junkb", [128, 1024], BF16)
    ss = [sb(es2, "ssb%d" % i, [128, 1]) for i in range(2)]
    rstd = [sb(es2, "rstdb%d" % i, [128, 1]) for i in range(2)]
    xn = [sb(es2, "xnb%d" % i, [128, 1024], BF16) for i in range(2)]
    hT = [sb(es2, "hTb%d" % i, [128, 8, 512], BF16) for i in range(2)]
    qbT = sb(es2, "qbT", [128, 4, 8, 128], BF16)
    kbT = sb(es2, "kbT", [128, 640], BF16)
    vba = [sb(es2, "vba%d" % i, [128, 2, 65], BF16) for i in range(3)]
    thb = sb(es2, "thb", [128, 1024], BF16)
    Pc = [sb(es2, "Pc%d" % i, [128, 4, 128], BF16) for i in range(2)]
    Pp = [sb(es2, "Pp%d" % i, [128, 4, 128], BF16) for i in range(2)]
    den = [sb(es2, "den%d" % i, [128, 4]) for i in range(2)]
    ob_sb = sb(es2, "ob_sb", [128, 16, 64])
    oa_in = [sb(es2, "oa_in%d" % i, [128, 1024], BF16) for i in range(2)]
    xr = [sb(es2, "xr%d" % i, [128, 1024]) for i in range(2)]
    merged = sb(es2, "merged", [128, 1024], BF16)
    mergedT = sb(es2, "mergedT", [128, 8, 128], BF16)
    hTtmp2 = sb(es2, "hTtmp2", [128, 8, 128])
    S.op("dve", ["wo", "gtb"], ["wo"], lambda e: e.tensor_tensor(
        out=wo[:, :, :], in0=wo[:, :, :], in1=gtb[:, 0, :].unsqueeze(1).to_broadcast([128, 8, 1024]), op=ALU.mult))
    for i in range(3):
        S.op("pool", [], [("vba", i)], lambda e: e.memset(vba[i][:, :, :], 1.0))
    S.op("pool", [], ["kbT"], lambda e: e.memset(kbT[:, :], 0.0))

    bufs2 = (xt, junk, ss, rstd, xn, p_tr)
    kctr[0] = 0
    ppq[0] = 0

    def A2(src, g, gi, tiles=(0, 1, 2, 3)):
        for t in tiles:
            r0 = g * 512 + t * 128
            ptr = normT(bufs2, src[r0:r0 + 128, :], None, sc1g, 0, "b", kctr[0])
            kctr[0] += 1
            mod_evac(ptr, lambda c: hT[gi % 2][:, c, t * 128:(t + 1) * 128], ("hT", gi % 2), sc1g, 0)

    def vb_proj(hb, hr, t, slot):
        pp, pr = tm_proj(wB, "wB", OVB, 128, hb, hr, t)
        S.op("act", [pr], [("vba", slot)], lambda e: e.activation(
            vba[slot][:, :, 0:64], pp[:, 0:128].rearrange("p (g d) -> p g d", g=2), AF.Copy))

    if NTP > 0:
        A2(xp, NGP - 1, 1, tiles=(3,))
        fm_proj(wB, "wB", OKB, 128, hT[1], ("hT", 1), 128, kbT[:, 0:128], "kbT", tok0=384)
        vb_proj(hT[1], ("hT", 1), 3, 0)

    thb2 = [thb, sb(es2, "thb2", [128, 1024], BF16)]

    def C2_loads(tg, row0):
        b2 = tg % 2
        S.dma("sp", "oain%d" % b2, [], [("oa_in", b2)], oa_in[b2][:, :], oa_s[row0:row0 + 128, :])
        S.dma("sp", "xr%d" % b2, [], [("xr", b2)], xr[b2][:, :], xm[row0:row0 + 128, :])

    def C2_P(gi, t, tg):
        hb, hr = hT[gi % 2], ("hT", gi % 2)
        vb_proj(hb, hr, t, (tg + 1) % 3)
        tb = thb2[tg % 2]
        for hf in range(2):
            sl = slice(hf * 512, (hf + 1) * 512)
            pp, pr = tm_proj(wB, "wB", OGB + hf * 512, 512, hb, hr, t)
            S.op("act", [pr], [("thb", tg % 2)], lambda e: e.activation(tb[:, sl], pp[:, :], AF.Tanh, scale=0.5))

    def C2_S(t, tg, u):
        g, j = u // 2, u % 2
        pb = u % 2
        ks = slice(g * 64, (g + 1) * 64)
        qv = qbT[ks, t, 4 * j:4 * j + 4, :]
        S.op("pe", ["kbT", "qbT"], [("p_sc", pb, 0)], lambda e: e.matmul(
            p_sc[pb][0][:, :], kbT[ks, 128 + t * 128:256 + t * 128], qv, start=True, stop=False), last=False)
        S.op("pe", ["ident_bf", "mbc"], [("p_sc", pb, 0)], lambda e: e.matmul(
            p_sc[pb][0][:, :], ident_bf[:, :], mbc[:, :, :].rearrange("p h q -> p (h q)"), start=False, stop=True))
        S.op("pe", ["kbT", "qbT"], [("p_sc", pb, 1)], lambda e: e.matmul(
            p_sc[pb][1][:, :], kbT[ks, t * 128:128 + t * 128], qv, start=True, stop=False), last=False)
        S.op("pe", ["ident_bf", "mbp"], [("p_sc", pb, 1)], lambda e: e.matmul(
            p_sc[pb][1][:, :], ident_bf[:, :], mbp[:, :, :].rearrange("p h q -> p (h q)"), start=False, stop=True))
        S.op("act", [("p_sc", pb, 0)], [("Pc", pb)], lambda e: e.activation(
            Pc[pb][:, :, :], p_sc[pb][0][:, :].rearrange("p (h q) -> p h q", h=4), AF.Exp, scale=0.125))
        if tg == 0:
            S.op("act", [("p_sc", pb, 1), "fbias"], [("Pp", pb)], lambda e: e.activation(
                Pp[pb][:, :, :], p_sc[pb][1][:, :].rearrange("p (h q) -> p h q", h=4), AF.Exp, scale=0.125,
                bias=fbias[:, 0:1]))
        else:
            S.op("act", [("p_sc", pb, 1)], [("Pp", pb)], lambda e: e.activation(
                Pp[pb][:, :, :], p_sc[pb][1][:, :].rearrange("p (h q) -> p h q", h=4), AF.Exp, scale=0.125))

    def C2_PV(tg, u):
        g, j = u // 2, u % 2
        pb = u % 2
        scur, sprev = (tg + 1) % 3, tg % 3
        for hh in range(4):
            S.op("pe", [("Pp", pb), ("vba", sprev)], ["p_ob"], lambda e: e.matmul(
                p_ob[:, hh, :], Pp[pb][:, hh, :], vba[sprev][:, g, :], start=True, stop=False), last=False)
            S.op("pe", [("Pc", pb), ("vba", scur)], ["p_ob"], lambda e: e.matmul(
                p_ob[:, hh, :], Pc[pb][:, hh, :], vba[scur][:, g, :], start=False, stop=True), last=(hh == 3))
        h0 = g * 8 + 4 * j
        S.op("dve", ["p_ob", "expsink"], [("den", pb)], lambda e: e.tensor_tensor(
            out=den[pb][:, :], in0=p_ob[:, :, 64], in1=expsink[:, h0:h0 + 4], op=ALU.add))
        S.op("dve", [("den", pb)], [("den", pb)], lambda e: e.reciprocal(den[pb][:, :], den[pb][:, :]))
        S.op("dve", ["p_ob", ("den", pb)], ["ob_sb"], lambda e: e.tensor_tensor(
            out=ob_sb[:, h0:h0 + 4, :], in0=p_ob[:, :, 0:64],
            in1=den[pb][:, :].unsqueeze(2).to_broadcast([128, 4, 64]), op=ALU.mult))

    def T_merge(tg):
        b2 = tg % 2
        tb = thb2[tg % 2]
        obf = ob_sb[:, :, :].rearrange("p h d -> p (h d)")
        S.op("dve", [("thb", tg % 2), "ob_sb"], ["ob_sb"], lambda e: e.scalar_tensor_tensor(
            out=obf, in0=tb[:, :], scalar=1.0, in1=obf, op0=ALU.add, op1=ALU.mult))
        S.op("dve", ["ob_sb", ("oa_in", b2)], ["merged"], lambda e: e.tensor_tensor(
            out=merged[:, :], in0=obf, in1=oa_in[b2][:, :], op=ALU.add))
        for c in range(8):
            S.op("pe", ["merged", "ident_bf"], ["p_tr"], lambda e: e.transpose(
                p_tr[:, c, :], merged[:, c * 128:(c + 1) * 128], ident_bf[:, :]), last=(c == 7))
        S.op("act", ["p_tr"], ["mergedT"], lambda e: e.activation(mergedT[:, :, :], p_tr[:, :, :], AF.Copy))

    def T_wo(tg, hf):
        b2 = tg % 2
        i = ppq[0] % 2
        ppq[0] += 1
        pp = p_proj[i]
        sl = slice(hf * 512, (hf + 1) * 512)
        for c in range(8):
            S.op("pe", ["mergedT", "wo"], [("pp", i)], lambda e: e.matmul(
                pp[:, :], mergedT[:, c, :], wo[:, c, sl], start=(c == 0), stop=(c == 7)), last=(c == 7))
        S.op("dve", [("pp", i), ("xr", b2)], [("xr", b2)], lambda e: e.tensor_tensor(
            out=xr[b2][:, sl], in0=pp[:, :], in1=xr[b2][:, sl], op=ALU.add))

    def T_end(tg, row0):
        b2 = tg % 2
        S.dma("sp", "x1o%d" % b2, [("xr", b2)], [], x1_s[row0:row0 + 128, :], xr[b2][:, :])

    A2(xm, 0, 0)
    prev = None
    if NGM > 1:
        xload(bufs2, xm[512:640, :], "b", kctr[0])
    C2_loads(0, 0)
    for g in range(NGM):
        hb, hr = hT[g % 2], ("hT", g % 2)
        for m in range(8):
            i = ppq[0] % 2
            ppq[0] += 1
            pp = p_proj[i]
            for c in range(8):
                S.op("pe", ["wB", hr], [("pp", i)], lambda e: e.matmul(
                    pp[:, :], wB[:, c, OQB + m * 128:OQB + (m + 1) * 128],
                    hb[:, c, :], start=(c == 0), stop=(c == 7)), last=(c == 7))
            evac_copy(qbT[:, :, m, :], pp[:, :].rearrange("p (t q) -> p t q", t=4), [("pp", i)], ["qbT"])
        fm_proj(wB, "wB", OKB, 128, hb, hr, 512, kbT[:, 128:640], "kbT")
        C2_P(g, 0, g * 4)
        for t in range(4):
            tg = g * 4 + t
            row0 = g * 512 + t * 128
            wd_some()
            ka_ = None
            if g + 1 < NGM:
                ka_ = kctr[0]
                kctr[0] += 1
                r1_ = (g + 1) * 512 + (t + 1) * 128
                if r1_ < M:
                    xload(bufs2, xm[r1_:r1_ + 128, :], "b", ka_ + 1)
                norm_n1(bufs2, "b", ka_)
            C2_S(t, tg, 0)
            C2_S(t, tg, 1)
            if ka_ is not None:
                norm_n2(bufs2, "b", ka_)
            if prev is not None:
                T_merge(prev[0])
            C2_PV(tg, 0)
            if ka_ is not None:
                norm_n3(bufs2, "b", ka_)
            C2_S(t, tg, 2)
            if prev is not None:
                T_wo(prev[0], 0)
            C2_PV(tg, 1)
            if ka_ is not None:
                norm_tr(bufs2, "b", ka_)
            C2_S(t, tg, 3)
            if ka_ is not None:
                norm_ev(bufs2, hT[(g + 1) % 2][:, :, t * 128:(t + 1) * 128], ("hT", (g + 1) % 2), hTtmp2, "hTtmp2")
            if prev is not None:
                T_wo(prev[0], 1)
                T_end(*prev)
            C2_PV(tg, 2)
            if tg + 1 < NTM:
                C2_loads(tg + 1, row0 + 128)
            C2_PV(tg, 3)
            if t < 3:
                C2_P(g, t + 1, tg + 1)
            prev = (tg, row0)
        S.op("pool", ["kbT"], ["kbT"], lambda e: e.tensor_copy(kbT[:, 0:128], kbT[:, 512:640]))
    T_merge(prev[0])
    T_wo(prev[0], 0)
    T_wo(prev[0], 1)
    T_end(*prev)

    S.barrier()
    es2.close()

    es3 = ExitStack()
    wgu = [sb(es3, "wgu%d" % i, [128, 8, 512], BF16) for i in range(3)]
    wrt = sb(es3, "wrt", [128, 8, 20]); brt = sb(es3, "brt", [1, 20]); ones1 = sb(es3, "ones1", [1, 128])
    sel = sb(es3, "sel", [16, 16, 128], BF16)
    gfb = sb(es3, "gfb", [128, 1024])
    x1t = [sb(es3, "x1c%d" % i, [128, 1024]) for i in range(2)]
    junk = sb(es3, "junkc", [128, 1024], BF16)
    ss = [sb(es3, "ssc%d" % i, [128, 1]) for i in range(2)]
    rstd = [sb(es3, "rstdc%d" % i, [128, 1]) for i in range(2)]
    xn2 = [sb(es3, "xn2_0", [128, 1024])] * 2
    h2f = [sb(es3, "h2f_0", [128, 8, 128])] * 2
    h2b = [sb(es3, "h2b%d" % i, [128, 8, 512], BF16) for i in range(2)]
    combT = [sb(es3, "combT%d" % i, [16, 512], BF16) for i in range(2)]
    cbc = [sb(es3, "cbc%d" % i, [128, 512], BF16) for i in range(2)]
    th = [sb(es3, "th%d" % i, [128, 512]) for i in range(2)]
    u1 = [sb(es3, "u1%d" % i, [128, 512]) for i in range(2)]
    hid = sb(es3, "hid", [128, 32, 512], BF16)
    xr = [sb(es3, "xrc%d" % i, [128, 1024]) for i in range(2)]
    x2 = [sb(es3, "x2%d" % i, [128, 1024]) for i in range(2)]
    sm = {n: sb(es3, "r_" + n, [128, w]) for n, w in [
        ("lg", 20), ("gmax", 1), ("gex", 4), ("gsum", 1), ("gw", 1), ("oh4", 4), ("pen", 16), ("msk", 16),
        ("m8", 8), ("oh1", 16), ("msk2", 16), ("m8b", 8), ("oh2", 16), ("dd", 1), ("w1", 1), ("w2", 1), ("comb", 16)]}
    p_tf = ps(es3, "p_tf", [128, 8, 128])
    p_small = ps(es3, "p_small", [128, 512])
    p_rt = p_small[:, 0:32]
    p_ct = p_small[0:16, 128:256]
    p_cbc = ps(es3, "p_cbc", [128, 512])
    p_gu = [ps(es3, "p_gu%d" % i, [128, 512]) for i in range(4)]

    S.dma("sp", "wrt", [], ["wrt"], wrt[:, :, :], w_rt.rearrange("(c p) n -> p c n", p=128))
    S.dma("sp", "brt", [], ["brt"], brt[:, :], b_rt[:, :])
    S.dma("sp", "gfb", [], ["gfb"], gfb[:, :], gf_bc[:, :])
    S.op("pool", [], ["ones1"], lambda e: e.memset(ones1[:, :], 1.0))
    S.op("pool", [], ["sel"], lambda e: e.memset(sel[:, :, :], 1.0))
    S.op("pool", ["sel"], ["sel"], lambda e: e.affine_select(
        out=sel[:, :, :], in_=sel[:, :, :], pattern=[[-1, 16], [0, 128]], compare_op=ALU.is_equal, fill=0.0,
        base=0, channel_multiplier=1))

    wq = [0]

    def load_expert(q):
        e_ = q % 16
        b = q % 3
        S.dma("sp", "wgu%d" % b, [], [("wgu", b)], wgu[b][:, :, :], wgu_s[e_])

    def dv(rd, wr, fn):
        S.op("dve", [("sm", r) for r in rd], [("sm", w) for w in wr], fn)

    k3 = [0]

    def A3_stages(g, gi):
        st = []
        for t in range(4):
            k = k3[0]
            k3[0] += 1
            st.extend(A3_tile(g, gi, t, k))
        return st

    def A3_tile(g, gi, t, k):
        b = k % 2
        r0 = g * 512 + t * 128
        lg = sm["lg"]

        def s1():
            if k == 0:
                S.dma("sp", "cx0", [], [("cx", 0)], x1t[0][:, :], x1_s[0:128, :])
            if (k + 1) * 128 < M:
                bn = (k + 1) % 2
                S.dma("sp", "cx%d" % bn, [], [("cx", bn)], x1t[bn][:, :], x1_s[(k + 1) * 128:(k + 2) * 128, :])
            S.op("act", [("cx", b)], ["cjunk", ("css", b)], lambda e: e.activation(
                junk[:, :], x1t[b][:, :], AF.Square, accum_out=ss[b][:, :]))
            S.op("dve", [("css", b)], [("css", b)], lambda e: e.tensor_scalar(
                out=ss[b][:, :], in0=ss[b][:, :], scalar1=1.0 / 1024.0, scalar2=EPS, op0=ALU.mult, op1=ALU.add))
            S.op("pool", [("css", b), "nhalf"], [("crs", b)], lambda e: e.tensor_tensor(
                out=rstd[b][:, :], in0=ss[b][:, :], in1=nhalf[:, 0:1], op=ALU.pow))
            S.op("dve", [("cx", b), ("crs", b)], ["xn2"], lambda e: e.tensor_scalar(
                out=xn2[b][:, :], in0=x1t[b][:, :], scalar1=rstd[b][:, 0:1], scalar2=None, op0=ALU.mult))

        def s2():
            for c in range(8):
                S.op("pe", ["xn2", "ident_f"], ["p_tf"], lambda e: e.transpose(
                    p_tf[:, c, :], xn2[b][:, c * 128:(c + 1) * 128], ident_f[:, :]), last=(c == 7))
            for c in range(8):
                S.op("dve", ["p_tf", "modT", "sc2g"], ["h2f"], lambda e: e.tensor_scalar(
                    out=h2f[b][:, c, :], in0=p_tf[:, c, :], scalar1=sc2g[:, c:c + 1], scalar2=modT[:, 24 + c:25 + c],
                    op0=ALU.mult, op1=ALU.add))
            S.op("pool", ["h2f"], [("h2b", gi % 2)], lambda e: e.tensor_copy(
                h2b[gi % 2][:, :, t * 128:(t + 1) * 128], h2f[b][:, :, :]))

        def s3():
            for c in range(8):
                S.op("pe", ["h2f", "wrt"], ["p_rt"], lambda e: e.matmul(
                    p_rt[:, 0:20], h2f[b][:, c, :], wrt[:, c, :], start=(c == 0), stop=False), last=False)
            S.op("pe", ["ones1", "brt"], ["p_rt"], lambda e: e.matmul(
                p_rt[:, 0:20], ones1[:, :], brt[:, :], start=False, stop=True))
            S.op("dve", ["p_rt"], [("sm", "lg")], lambda e: e.tensor_copy(lg[:, :], p_rt[:, 0:20]))
            dv(["lg"], ["gmax"], lambda e: e.tensor_reduce(
                out=sm["gmax"][:, :], in_=lg[:, 0:4], axis=mybir.AxisListType.X, op=ALU.max))
            dv(["lg", "gmax"], ["gex"], lambda e: e.tensor_scalar(
                out=sm["gex"][:, :], in0=lg[:, 0:4], scalar1=sm["gmax"][:, 0:1], scalar2=None, op0=ALU.subtract))
            S.op("act", [("sm", "gex")], [("sm", "gex"), ("sm", "gsum")], lambda e: e.activation(
                sm["gex"][:, :], sm["gex"][:, :], AF.Exp, accum_out=sm["gsum"][:, :]))
            dv(["gsum"], ["gw"], lambda e: e.reciprocal(sm["gw"][:, :], sm["gsum"][:, :]))
            dv(["lg", "gmax"], ["oh4"], lambda e: e.tensor_scalar(
                out=sm["oh4"][:, :], in0=lg[:, 0:4], scalar1=sm["gmax"][:, 0:1], scalar2=None, op0=ALU.is_equal))
            dv(["oh4"], ["pen"], lambda e: e.tensor_scalar(
                out=sm["pen"][:, :].rearrange("p (g j) -> p g j", g=4),
                in0=sm["oh4"][:, :].unsqueeze(2).to_broadcast([128, 4, 4]),
                scalar1=-1.0, scalar2=1.0e9, op0=ALU.add, op1=ALU.mult))
            dv(["lg", "pen"], ["msk"], lambda e: e.tensor_tensor(
                out=sm["msk"][:, :], in0=lg[:, 4:20], in1=sm["pen"][:, :], op=ALU.add))
            dv(["msk"], ["m8"], lambda e: e.max(sm["m8"][:, :], sm["msk"][:, :]))
            dv(["msk", "m8"], ["oh1"], lambda e: e.tensor_scalar(
                out=sm["oh1"][:, :], in0=sm["msk"][:, :], scalar1=sm["m8"][:, 0:1], scalar2=None, op0=ALU.is_equal))
            dv(["msk", "m8"], ["oh2"], lambda e: e.tensor_scalar(
                out=sm["oh2"][:, :], in0=sm["msk"][:, :], scalar1=sm["m8"][:, 1:2], scalar2=None, op0=ALU.is_equal))
            dv(["m8"], ["dd"], lambda e: e.tensor_tensor(
                out=sm["dd"][:, :], in0=sm["m8"][:, 1:2], in1=sm["m8"][:, 0:1], op=ALU.subtract))
            S.op("act", [("sm", "dd")], [("sm", "dd")], lambda e: e.activation(sm["dd"][:, :], sm["dd"][:, :], AF.Exp))
            dv(["dd"], ["w1"], lambda e: e.tensor_scalar(
                out=sm["w1"][:, :], in0=sm["dd"][:, :], scalar1=1.0, scalar2=None, op0=ALU.add))
            dv(["w1"], ["w1"], lambda e: e.reciprocal(sm["w1"][:, :], sm["w1"][:, :]))
            dv(["w1", "dd"], ["w2"], lambda e: e.tensor_tensor(
                out=sm["w2"][:, :], in0=sm["w1"][:, :], in1=sm["dd"][:, :], op=ALU.mult))
            dv(["w1", "gw"], ["w1"], lambda e: e.tensor_tensor(
                out=sm["w1"][:, :], in0=sm["w1"][:, :], in1=sm["gw"][:, :], op=ALU.mult))
            dv(["w2", "gw"], ["w2"], lambda e: e.tensor_tensor(
                out=sm["w2"][:, :], in0=sm["w2"][:, :], in1=sm["gw"][:, :], op=ALU.mult))
            dv(["oh1", "w1"], ["comb"], lambda e: e.tensor_scalar(
                out=sm["comb"][:, :], in0=sm["oh1"][:, :], scalar1=sm["w1"][:, 0:1], scalar2=None, op0=ALU.mult))
            dv(["oh2", "w2", "comb"], ["comb"], lambda e: e.scalar_tensor_tensor(
                out=sm["comb"][:, :], in0=sm["oh2"][:, :], scalar=sm["w2"][:, 0:1], in1=sm["comb"][:, :],
                op0=ALU.mult, op1=ALU.add))

        def s4():
            S.op("pe", [("sm", "comb"), "ident_f"], ["p_rt"], lambda e: e.transpose(
                p_ct, sm["comb"][:, :], ident_f[:, :]))
            S.op("act", ["p_rt"], [("combT", gi % 2)], lambda e: e.activation(
                combT[gi % 2][:, t * 128:(t + 1) * 128], p_ct, AF.Copy))
        return [s1, s2, s3, s4]

    NG = NGM
    load_expert(0)
    load_expert(1)
    load_expert(2)
    for f in A3_stages(0, 0):
        f()
    for g in range(NG):
        hb, hr = h2b[g % 2], ("h2b", g % 2)
        nxt = A3_stages(g + 1, g + 1) if g + 1 < NG else []
        for e_ in range(16):
            q = g * 16 + e_
            b = q % 3
            cb = q % 2
            S.op("pe", ["sel", ("combT", g % 2)], ["p_cbc"], lambda e: e.matmul(
                p_cbc[:, :], sel[:, e_, :], combT[g % 2][:, :], start=True, stop=True))
            S.op("act", ["p_cbc"], [("cbc", cb)], lambda e: e.activation(cbc[cb][:, :], p_cbc[:, :], AF.Copy))
            for fc in range(2):
                pg, pu = p_gu[fc * 2], p_gu[fc * 2 + 1]
                rg, ru = ("p_gu", fc * 2), ("p_gu", fc * 2 + 1)
                for c in range(8):
                    S.op("pe", [("wgu", b), hr], [rg], lambda e: e.matmul(
                        pg[:, :], wgu[b][:, c, fc * 128:(fc + 1) * 128], hb[:, c, :],
                        start=(c == 0), stop=(c == 7)), last=(c == 7))
                for c in range(8):
                    S.op("pe", [("wgu", b), hr], [ru], lambda e: e.matmul(
                        pu[:, :], wgu[b][:, c, 256 + fc * 128:256 + (fc + 1) * 128], hb[:, c, :],
                        start=(c == 0), stop=(c == 7)), last=(c == 7))
                S.op("act", [rg], [("th", fc)], lambda e: e.activation(th[fc][:, :], pg[:, :], AF.Tanh, scale=0.5))
                S.op("dve", [rg, ("th", fc)], [("th", fc)], lambda e: e.scalar_tensor_tensor(
                    out=th[fc][:, :], in0=th[fc][:, :], scalar=1.0, in1=pg[:, :], op0=ALU.add, op1=ALU.mult))
                S.op("dve", [ru, ("th", fc)], [("u1", fc)], lambda e: e.tensor_tensor(
                    out=u1[fc][:, :], in0=th[fc][:, :], in1=pu[:, :], op=ALU.mult))
                S.op("pool", [("u1", fc), ("cbc", cb)], [("hid", e_ * 2 + fc)], lambda e: e.tensor_tensor(
                    out=hid[:, e_ * 2 + fc, :], in0=u1[fc][:, :], in1=cbc[cb][:, :], op=ALU.mult))
            if q + 3 < NG * 16:
                load_expert(q + 3)
            if e_ < len(nxt):
                nxt[e_]()
        for t in range(4):
            k = g * 4 + t
            b = k % 2
            r0 = g * 512 + t * 128
            S.dma("sp", "xrc%d" % b, [], [("xrc", b)], xr[b][:, :], x1_s[r0:r0 + 128, :])
            for hf in range(2):
                pi = (k * 2 + hf) % 4
                py = p_gu[pi]
                sl = slice(hf * 512, (hf + 1) * 512)
                for ef in range(32):
                    S.op("pe", [("hid", ef), "wd"], [("p_gu", pi)], lambda e: e.matmul(
                        py[:, :], hid[:, ef, t * 128:(t + 1) * 128], wd[:, ef // 2, ef % 2, sl],
                        start=(ef == 0), stop=(ef == 31)), last=(ef == 31))
                S.op("dve", [("p_gu", pi), "gtb"], [("x2", b)], lambda e: e.tensor_tensor(
                    out=x2[b][:, sl], in0=py[:, :], in1=gtb[:, 1, sl], op=ALU.mult))
                S.op("pool", [("x2", b), ("xrc", b)], [("x2", b)], lambda e: e.tensor_tensor(
                    out=x2[b][:, sl], in0=x2[b][:, sl], in1=xr[b][:, sl], op=ALU.add))
            S.op("act", [("x2", b)], ["cjunk", ("fss", b)], lambda e: e.activation(
                junk[:, :], x2[b][:, :], AF.Square, accum_out=ss[b][:, :]))
            S.op("dve", [("fss", b)], [("fss", b)], lambda e: e.tensor_scalar(
                out=ss[b][:, :], in0=ss[b][:, :], scalar1=1.0 / 1024.0, scalar2=EPS, op0=ALU.mult, op1=ALU.add))
            S.op("pool", [("fss", b), "nhalf"], [("frs", b)], lambda e: e.tensor_tensor(
                out=rstd[b][:, :], in0=ss[b][:, :], in1=nhalf[:, 0:1], op=ALU.pow))
            S.op("dve", [("x2", b), ("frs", b), "gfb"], [("x2", b)], lambda e: e.scalar_tensor_tensor(
                out=x2[b][:, :], in0=x2[b][:, :], scalar=rstd[b][:, 0:1], in1=gfb[:, :], op0=ALU.mult, op1=ALU.mult))
            S.dma("sp", "out%d" % b, [("x2", b)], [], out[r0:r0 + 128, :], x2[b][:, :])

    S.finish("sp")
    es3.close()
    es_w.close()
    es0.close()
    return nc


def make_in_maps(inputs, n_cores, NTM, NTP, seq):
    f = lambda a: np.ascontiguousarray(np.asarray(a, dtype=np.float32))
    x = f(inputs["x"]); c = f(inputs["c"])
    w_ada = f(inputs["w_ada"][0]); b_ada = f(inputs["b_ada"][0])
    pm = lambda v: np.ascontiguousarray(v.reshape(-1, 128).T)
    rep = lambda v: np.ascontiguousarray(np.broadcast_to(v[None, :], (128, v.shape[0])))
    shared = {
        "w_ada": w_ada, "bada_pm": pm(b_ada),
        "bada_bc": np.ascontiguousarray(np.stack([rep(b_ada[2048:3072]), rep(b_ada[5120:6144])], axis=1)),
        "g1_pm": pm(f(inputs["norm1_g"][0])), "g2_pm": pm(f(inputs["norm2_g"][0])),
        "gf_bc": rep(f(inputs["norm_f_g"])),
        "w_in": f(inputs["w_in"][0]),
        "wgk": np.ascontiguousarray(np.concatenate([f(inputs["w_gk2"][0]), f(inputs["b_gk"][0])[None, :]], axis=0)),
        "gng_bc": rep(np.tile(f(inputs["gla_norm_g"][0]), 4)),
        "sink_bc": rep(f(inputs["sink"][0])),
        "w_o": f(inputs["w_o"][0]),
        "w_rt": np.ascontiguousarray(np.concatenate([f(inputs["w_group"][0]), f(inputs["w_router"][0])], axis=1)),
        "b_rt": np.ascontiguousarray(np.concatenate([f(inputs["b_group"][0]), f(inputs["b_router"][0])])[None, :]),
        "w_gate": f(inputs["w_gate"][0]), "w_up": f(inputs["w_up"][0]), "w_down": f(inputs["w_down"][0]),
    }
    M, P = NTM * 128, NTP * 128
    maps = []
    for k in range(n_cores):
        b, half = k // 2, k % 2
        m = dict(shared)
        m["xm"] = np.ascontiguousarray(x[b, half * M:(half + 1) * M])
        if half == 1:
            m["xp"] = np.ascontiguousarray(x[b, 0:P])
        else:
            m["xp"] = np.zeros((P, 1024), np.float32)
        m["flag"] = np.full((128, 1), float(half), np.float32)
        m["c_pm"] = pm(c[b])
        maps.append(m)
    return maps


def kernel(**inputs):
    NTM = NTP = 32
    nc = bass.Bass("TRN2", target_bir_lowering=False)
    build(nc, NTM, NTP)
    maps = make_in_maps(inputs, 8, NTM, NTP, 8192)
    res = run_bass_kernel_spmd(nc, maps, core_ids=list(range(8)))
    out = np.zeros((4, 8192, 1024), np.float32)
    for k in range(8):
        b, half = k // 2, k % 2
        out[b, half * 4096:(half + 1) * 4096] = res.results[k]["out"]
    return out
```

```python
import numpy as np
from contextlib import ExitStack
import concourse.bass as bass
import concourse.mybir as mybir
from concourse.bass_utils import run_bass_kernel_spmd

F32 = mybir.dt.float32
BF16 = mybir.dt.bfloat16
AF = mybir.ActivationFunctionType
ALU = mybir.AluOpType

EPS = 1e-6
EPS_GLA = 1e-6 * 128.0


class Sched:
    def __init__(self, nc):
        self.nc = nc
        self.eng = {"pe": nc.tensor, "act": nc.scalar, "dve": nc.vector, "pool": nc.gpsimd, "sp": nc.sync}
        self.sem, self.cnt, self.seen, self.pend = {}, {}, {}, {}
        for e in self.eng:
            self.sem[e] = nc.alloc_semaphore("s_" + e)
            self.cnt[e] = 0
            self.seen[e] = {}
            self.pend[e] = []
        self.last_w, self.readers, self.dma_sems = {}, {}, {}

    def _wait(self, e, tok):
        sem, val, src = tok
        if src == e and e == "pe":
            return
        key = id(sem)
        if self.seen[e].get(key, 0) >= val:
            return
        self.seen[e][key] = val
        self.eng[e].wait_ge(sem, val)

    def deps(self, e, reads, writes):
        for r in reads:
            t = self.last_w.get(r)
            if t is not None:
                self._wait(e, t)
        for w in writes:
            t = self.last_w.get(w)
            if t is not None:
                self._wait(e, t)
            for t in self.readers.get(w, ()):
                self._wait(e, t)

    def commit(self, tok, reads, writes):
        for r in reads:
            self.readers.setdefault(r, []).append(tok)
        for w in writes:
            self.last_w[w] = tok
            self.readers[w] = []

    def op(self, e, reads, writes, fn, last=True):
        self.deps(e, reads, writes)
        ins = fn(self.eng[e])
        self.pend[e].append((reads, writes))
        if last:
            self.cnt[e] += 1
            ins.then_inc(self.sem[e], 1)
            tok = (self.sem[e], self.cnt[e], e)
            for (r, w) in self.pend[e]:
                self.commit(tok, r, w)
            self.pend[e] = []

    def dma(self, q, semname, reads, writes, out, in_):
        self.deps(q, reads, writes)
        if semname not in self.dma_sems:
            self.dma_sems[semname] = [self.nc.alloc_semaphore("d_" + semname), 0]
        ds = self.dma_sems[semname]
        ds[1] += 16
        self.eng[q].dma_start(out=out, in_=in_).then_inc(ds[0], 16)
        self.commit((ds[0], ds[1], "dma"), reads, writes)

    def barrier(self):
        toks = [(self.sem[e], self.cnt[e], e) for e in self.eng if self.cnt[e] > 0]
        toks += [(s, v, "dma") for (s, v) in self.dma_sems.values()]
        for e in self.eng:
            for t in toks:
                if t[2] == e:
                    continue
                self._wait(e, t)
        self.last_w.clear()
        self.readers.clear()

    def finish(self, e="sp"):
        for (sem, val) in self.dma_sems.values():
            self._wait(e, (sem, val, "dma"))


def build(nc, NTM, NTP):
    M, P = NTM * 128, NTP * 128
    NGM, NGP = NTM // 4, NTP // 4

    def dr(name, shape, dt=F32, kind="ExternalInput"):
        return nc.dram_tensor(name, shape, dt, kind=kind).ap()

    xm = dr("xm", [M, 1024]); xp = dr("xp", [P, 1024]); flag = dr("flag", [128, 1])
    c_pm = dr("c_pm", [128, 8]); w_ada = dr("w_ada", [1024, 6144]); bada_pm = dr("bada_pm", [128, 48])
    bada_bc = dr("bada_bc", [128, 2, 1024])
    g1_pm = dr("g1_pm", [128, 8]); g2_pm = dr("g2_pm", [128, 8]); gf_bc = dr("gf_bc", [128, 1024])
    w_in = dr("w_in", [1024, 6416]); wgk = dr("wgk", [17, 512]); gng_bc = dr("gng_bc", [128, 1024])
    sink_bc = dr("sink_bc", [128, 16]); w_o = dr("w_o", [1024, 1024])
    w_rt = dr("w_rt", [1024, 20]); b_rt = dr("b_rt", [1, 20])
    w_gate = dr("w_gate", [16, 1024, 256]); w_up = dr("w_up", [16, 1024, 256]); w_down = dr("w_down", [16, 256, 1024])
    out = dr("out", [M, 1024], kind="ExternalOutput")
    oa_s = dr("oa_s", [M, 1024], BF16, kind="Internal")
    x1_s = dr("x1_s", [M, 1024], F32, kind="Internal")
    wgu_s = dr("wgu_s", [16, 128, 8, 512], BF16, kind="Internal")
    wB_s = dr("wB_s", [128, 8, 2304], BF16, kind="Internal")
    wo_s = dr("wo_s", [128, 8, 1024], BF16, kind="Internal")

    S = Sched(nc)
    es0 = ExitStack()

    def sb(es, name, shape, dt=F32):
        return es.enter_context(nc.sbuf_tensor(name, shape, dt))

    def ps(es, name, shape, dt=F32):
        return es.enter_context(nc.psum_tensor(name, shape, dt))

    ident_bf = sb(es0, "ident_bf", [128, 128], BF16)
    ident_f = sb(es0, "ident_f", [128, 128], F32)
    tri = sb(es0, "tri", [128, 128], F32)
    maskg = sb(es0, "maskg", [128, 128], F32)
    mcur = sb(es0, "mcur", [128, 128], BF16)
    mprev = sb(es0, "mprev", [128, 128], BF16)
    c_sb = sb(es0, "c_sb", [128, 8]); scc = sb(es0, "scc", [128, 8], BF16)
    sc_bc = sb(es0, "sc_bc", [128, 8, 128], BF16)
    modT = sb(es0, "modT", [128, 48]); badap = sb(es0, "badap", [128, 48])
    g1s = sb(es0, "g1s", [128, 8]); g2s = sb(es0, "g2s", [128, 8])
    sc1g = sb(es0, "sc1g", [128, 8]); sc2g = sb(es0, "sc2g", [128, 8])
    gtb = sb(es0, "gtb", [128, 2, 1024])
    flag_sb = sb(es0, "flag_sb", [128, 1]); fbias = sb(es0, "fbias", [128, 1])
    nhalf = sb(es0, "nhalf", [128, 4])
    expsink = sb(es0, "expsink", [128, 16])

    def tri_fill(t, val, pattern, cm, cmp):
        tn = "const%d" % id(t)
        S.op("pool", [], [tn], lambda e: e.memset(t[:, :], val))
        S.op("pool", [tn], [tn], lambda e: e.affine_select(
            out=t[:, :], in_=t[:, :], pattern=pattern, compare_op=cmp, fill=0.0, base=0, channel_multiplier=cm))

    tri_fill(ident_bf, 1.0, [[-1, 128]], 1, ALU.is_equal)
    tri_fill(ident_f, 1.0, [[-1, 128]], 1, ALU.is_equal)
    tri_fill(tri, -1.0 / 16.0, [[1, 128]], -1, ALU.is_ge)
    tri_fill(maskg, 1.0, [[1, 128]], -1, ALU.is_ge)
    tri_fill(mcur, 1.0, [[1, 128]], -1, ALU.is_ge)
    tri_fill(mprev, 1.0, [[-1, 128]], 1, ALU.is_gt)
    S.op("pool", [], ["nhalf"], lambda e: e.memset(nhalf[:, :], -0.5))
    mbc = sb(es0, "mbc", [128, 4, 128], BF16)
    mbp = sb(es0, "mbp", [128, 4, 128], BF16)
    S.op("pool", [], ["mbc"], lambda e: e.memset(mbc[:, :, :], 0.0))
    S.op("pool", ["mbc"], ["mbc"], lambda e: e.affine_select(
        out=mbc[:, :, :], in_=mbc[:, :, :], pattern=[[0, 4], [1, 128]], compare_op=ALU.is_ge, fill=-30000.0,
        base=0, channel_multiplier=-1))
    S.op("pool", [], ["mbp"], lambda e: e.memset(mbp[:, :, :], 0.0))
    S.op("pool", ["mbp"], ["mbp"], lambda e: e.affine_select(
        out=mbp[:, :, :], in_=mbp[:, :, :], pattern=[[0, 4], [-1, 128]], compare_op=ALU.is_gt, fill=-30000.0,
        base=0, channel_multiplier=1))

    S.dma("sp", "c0", [], ["c_sb"], c_sb[:, :], c_pm[:, :])
    S.dma("sp", "c1", [], ["badap"], badap[:, :], bada_pm[:, :])
    S.dma("sp", "c2", [], ["g1s"], g1s[:, :], g1_pm[:, :])
    S.dma("sp", "c3", [], ["g2s"], g2s[:, :], g2_pm[:, :])
    S.dma("sp", "c4", [], ["gtb"], gtb[:, :, :], bada_bc[:, :, :])
    S.dma("sp", "c5", [], ["flag_sb"], flag_sb[:, :], flag[:, :])
    S.dma("sp", "c6", [], ["expsink"], expsink[:, :], sink_bc[:, :])
    S.op("act", ["expsink"], ["expsink"], lambda e: e.activation(expsink[:, :], expsink[:, :], AF.Exp))
    S.op("act", ["c_sb"], ["scc"], lambda e: e.activation(scc[:, :], c_sb[:, :], AF.Silu))
    S.op("dve", ["scc"], ["sc_bc"], lambda e: e.tensor_copy(
        sc_bc[:, :, :], scc[:, :].unsqueeze(2).to_broadcast([128, 8, 128])))
    S.op("dve", ["flag_sb"], ["fbias"], lambda e: e.tensor_scalar(
        out=fbias[:, :], in0=flag_sb[:, :], scalar1=-1.0, scalar2=30000.0, op0=ALU.add, op1=ALU.mult))

    es1 = ExitStack()
    wA = sb(es1, "wA", [128, 8, 4112], BF16)
    OQA, OKA, OVA, ORA, OZA, OGA = 0, 512, 1024, 2048, 3072, 3088
    wa_blk = [sb(es1, "wa_blk%d" % i, [128, 8, 512], BF16) for i in range(2)]
    wgk_sb = sb(es1, "wgk_sb", [17, 512], BF16)
    gng = sb(es1, "gng", [128, 1024])
    p_tr = ps(es1, "p_tr", [128, 8, 128], BF16)
    p_proj = [ps(es1, "p_proj%d" % i, [128, 512]) for i in range(2)]
    p_b = ps(es1, "p_b", [128, 4, 128])
    p_mod = p_b[:, 0, 0:48]
    p_att = ps(es1, "p_att", [128, 4, 128])
    p_o = ps(es1, "p_o", [128, 1024])
    p_T = ps(es1, "p_T", [128, 2, 256])

    w_in_r = w_in.rearrange("(c p) n -> p c n", p=128)
    w_ada_r = w_ada.rearrange("(c p) n -> p c n", p=128)

    def mod_dma(blk):
        b = blk % 2
        S.dma("pool", "wa%d" % b, [], [("wa", b)], wa_blk[b][:, :, :], w_ada_r[:, :, blk * 512:(blk + 1) * 512])

    def mod_compute(blk):
        b = blk % 2
        v = blk // 2
        if v in (2, 5):
            vi = 0 if v == 2 else 1
            hf = blk % 2
            pp = p_proj[hf]
            for c in range(8):
                S.op("pe", ["sc_bc", ("wa", b)], [("pp", hf)], lambda e: e.matmul(
                    pp[:, :], sc_bc[:, c, :], wa_blk[b][:, c, :], start=(c == 0), stop=(c == 7)), last=(c == 7))
            sl = gtb[:, vi, hf * 512:(hf + 1) * 512]
            S.op("dve", [("pp", hf), "gtb"], ["gtb"], lambda e: e.tensor_tensor(out=sl, in0=pp[:, :], in1=sl, op=ALU.add))
            S.op("dve", ["gtb"], ["gtb"], lambda e: e.tensor_scalar(
                out=sl, in0=sl, scalar1=0.5, scalar2=None, op0=ALU.mult))
        else:
            for q in range(4):
                j = blk * 4 + q
                for c in range(8):
                    S.op("pe", ["scc", ("wa", b)], ["p_b"], lambda e: e.matmul(
                        p_mod[:, j:j + 1], wa_blk[b][:, c, q * 128:(q + 1) * 128], scc[:, c:c + 1],
                        start=(c == 0), stop=(c == 7)), last=(c == 7))
            j0 = blk * 4
            S.op("dve", ["p_b", "badap"], ["modT"], lambda e: e.tensor_tensor(
                out=modT[:, j0:j0 + 4], in0=p_mod[:, j0:j0 + 4], in1=badap[:, j0:j0 + 4], op=ALU.add))

    mod_dma(0)
    mod_dma(1)
    for blk in range(4):
        mod_compute(blk)
        if blk + 2 < 4:
            mod_dma(blk + 2)
    S.op("dve", ["modT", "g1s"], ["sc1g"], lambda e: e.scalar_tensor_tensor(
        out=sc1g[:, :], in0=modT[:, 8:16], scalar=1.0, in1=g1s[:, :], op0=ALU.add, op1=ALU.mult))
    mod_next = [4]

    def mod_deferred():
        blk = mod_next[0]
        if blk >= 12:
            return
        mod_next[0] += 1
        mod_compute(blk)
        if blk + 2 < 12:
            mod_dma(blk + 2)
        if blk == 11:
            S.op("dve", ["modT", "g2s"], ["sc2g"], lambda e: e.scalar_tensor_tensor(
                out=sc2g[:, :], in0=modT[:, 32:40], scalar=1.0, in1=g2s[:, :], op0=ALU.add, op1=ALU.mult))

    def normT(es_bufs, src_rows, hT_ap_fn, scg, sh_off, tag, k):
        xt, junk, ss, rstd, xn, ptr = es_bufs
        b = k % 2
        b3 = k % len(xt)
        S.dma("sp", tag + "x%d" % b3, [], [(tag + "xt", b3)], xt[b3][:, :], src_rows)
        S.op("act", [(tag + "xt", b3)], [tag + "junk", (tag + "ss", b)], lambda e: e.activation(
            junk[:, :], xt[b3][:, :], AF.Square, accum_out=ss[b][:, :]))
        S.op("dve", [(tag + "ss", b)], [(tag + "ss", b)], lambda e: e.tensor_scalar(
            out=ss[b][:, :], in0=ss[b][:, :], scalar1=1.0 / 1024.0, scalar2=EPS, op0=ALU.mult, op1=ALU.add))
        S.op("pool", [(tag + "ss", b), "nhalf"], [(tag + "rstd", b)], lambda e: e.tensor_tensor(
            out=rstd[b][:, :], in0=ss[b][:, :], in1=nhalf[:, 0:1], op=ALU.pow))
        S.op("dve", [(tag + "xt", b3), (tag + "rstd", b)], [(tag + "xn", b)], lambda e: e.tensor_scalar(
            out=xn[b][:, :], in0=xt[b3][:, :], scalar1=rstd[b][:, 0:1], scalar2=None, op0=ALU.mult))
        for c in range(8):
            S.op("pe", [(tag + "xn", b), "ident_bf"], ["p_tr"], lambda e: e.transpose(
                ptr[:, c, :], xn[b][:, c * 128:(c + 1) * 128], ident_bf[:, :]), last=(c == 7))
        return ptr

    def mod_evac(ptr, dst_fn, dst_res, scg, sh_off):
        for c in range(8):
            S.op("dve", ["p_tr", "modT", "sc1g", "sc2g"], [dst_res], lambda e: e.tensor_scalar(
                out=dst_fn(c), in0=ptr[:, c, :], scalar1=scg[:, c:c + 1], scalar2=modT[:, sh_off + c:sh_off + c + 1],
                op0=ALU.mult, op1=ALU.add))

    xt = [sb(es1, "xt%d" % i, [128, 1024]) for i in range(2)]
    junk = sb(es1, "junk", [128, 1024], BF16)
    ss = [sb(es1, "ss%d" % i, [128, 1]) for i in range(2)]
    rstd = [sb(es1, "rstd%d" % i, [128, 1]) for i in range(2)]
    xn = [sb(es1, "xn%d" % i, [128, 1024], BF16) for i in range(2)]
    hT = [sb(es1, "hT%d" % i, [128, 8, 512], BF16) for i in range(2)]
    qaT = sb(es1, "qaT", [128, 4, 512], BF16)
    kaT = sb(es1, "kaT", [128, 4, 512], BF16)
    zT = [sb(es1, "zT%d" % i, [17, 512], BF16) for i in range(2)]
    e_sb = sb(es1, "e_sb", [128, 4, 512])
    l_sb = sb(es1, "l_sb", [128, 4, 512])
    va_sb = [sb(es1, "va_sb%d" % i, [128, 1024], BF16) for i in range(2)]
    thr = sb(es1, "thr", [128, 1024], BF16)
    t1 = sb(es1, "t1", [128, 1024], BF16)
    E_sb = sb(es1, "E_sb", [128, 4, 128]); Einv_sb = sb(es1, "Einv_sb", [128, 4, 128])
    qdecT = sb(es1, "qdecT", [128, 4, 128], BF16); kinvT = sb(es1, "kinvT", [128, 4, 128], BF16)
    kinv_tok = sb(es1, "kinv_tok", [128, 4, 128], BF16); attT_sb = sb(es1, "attT_sb", [128, 4, 128], BF16)
    St = sb(es1, "St", [128, 4, 256]); S_bf = sb(es1, "S_bf", [128, 4, 256], BF16)
    tmpS = sb(es1, "tmpS", [128, 4, 256])
    hTtmp = sb(es1, "hTtmp", [128, 8, 128])
    ssq = sb(es1, "ssq", [128, 4]); rso = sb(es1, "rso", [128, 4])
    otmp = sb(es1, "otmp", [128, 1024])
    oag = [sb(es1, "oag%d" % i, [128, 1024], BF16) for i in range(2)]

    for i in range(2):
        S.op("pool", [], [("zT", i)], lambda e: e.memset(zT[i][:, :], 1.0))
    S.op("pool", [], ["St"], lambda e: e.memset(St[:, :, :], 0.0))
    S.op("pool", [], ["S_bf"], lambda e: e.memset(S_bf[:, :, :], 0.0))

    bufs1 = (xt, junk, ss, rstd, xn, p_tr)
    kctr = [0]

    def A1(src, g, gi):
        for t in range(4):
            r0 = g * 512 + t * 128
            ptr = normT(bufs1, src[r0:r0 + 128, :], None, sc1g, 0, "a", kctr[0])
            kctr[0] += 1
            mod_evac(ptr, lambda c: hT[gi % 2][:, c, t * 128:(t + 1) * 128], ("hT", gi % 2), sc1g, 0)

    evq = [0]

    def evac_copy(dst, src, reads, writes):
        evq[0] += 1
        if evq[0] % 2 == 0:
            S.op("act", reads, writes, lambda e: e.activation(dst, src, AF.Copy))
        else:
            S.op("dve", reads, writes, lambda e: e.tensor_copy(dst, src))

    ppq = [0]

    def fm_proj(w_t, wres, col0, ncol, hbuf, hres, ntok, dst, dres, tok0=0):
        i = ppq[0] % 2
        ppq[0] += 1
        pp = p_proj[i]
        for c in range(8):
            S.op("pe", [wres, hres], [("pp", i)], lambda e: e.matmul(
                pp[0:ncol, 0:ntok], w_t[:, c, col0:col0 + ncol], hbuf[:, c, tok0:tok0 + ntok],
                start=(c == 0), stop=(c == 7)), last=(c == 7))
        evac_copy(dst, pp[0:ncol, 0:ntok], [("pp", i)], [dres])

    def tm_proj(w_t, wres, col0, ncol, hbuf, hres, t):
        i = ppq[0] % 2
        ppq[0] += 1
        pp = p_proj[i]
        for c in range(8):
            S.op("pe", [wres, hres], [("pp", i)], lambda e: e.matmul(
                pp[:, 0:ncol], hbuf[:, c, t * 128:(t + 1) * 128], w_t[:, c, col0:col0 + ncol],
                start=(c == 0), stop=(c == 7)), last=(c == 7))
        return pp, ("pp", i)

    def B1(gi, is_main):
        hb, hr = hT[gi % 2], ("hT", gi % 2)
        for h in range(4):
            fm_proj(wA, "wA_k", OKA + h * 128, 128, hb, hr, 512, kaT[:, h, :], "kaT")
        if is_main:
            for h in range(4):
                fm_proj(wA, "wA_q", OQA + h * 128, 128, hb, hr, 512, qaT[:, h, :], "qaT")
        fm_proj(wA, "wA_k", OZA, 16, hb, hr, 512, zT[gi % 2][0:16, :], ("zT", gi % 2))
        for t in range(4):
            i = ppq[0] % 2
            ppq[0] += 1
            pp = p_proj[i]
            S.op("pe", [("zT", gi % 2), "wgk_sb"], [("pp", i)], lambda e: e.matmul(
                pp[:, :], zT[gi % 2][:, t * 128:(t + 1) * 128], wgk_sb[:, :], start=True, stop=True))
            S.op("act", [("pp", i)], [("e_sb", t)], lambda e: e.activation(e_sb[:, t, :], pp[:, :], AF.Exp, scale=-1.0))
        for t in range(4):
            S.op("act", [("e_sb", t)], [("l_sb", t)], lambda e: e.activation(l_sb[:, t, :], e_sb[:, t, :], AF.Ln, bias=1.0))

    def xload(bufs, src_rows, tag, k):
        b3 = k % len(bufs[0])
        S.dma("sp", tag + "x%d" % b3, [], [(tag + "xt", b3)], bufs[0][b3][:, :], src_rows)

    def norm_n1(bufs, tag, k):
        xt_, junk_, ss_, rstd_, xn_, ptr_ = bufs
        b = k % 2
        b3 = k % len(xt_)
        S.op("act", [(tag + "xt", b3)], [tag + "junk", (tag + "ss", b)], lambda e: e.activation(
            junk_[:, :], xt_[b3][:, :], AF.Square, accum_out=ss_[b][:, :]))

    def norm_n2(bufs, tag, k):
        xt_, junk_, ss_, rstd_, xn_, ptr_ = bufs
        b = k % 2
        S.op("dve", [(tag + "ss", b)], [(tag + "ss", b)], lambda e: e.tensor_scalar(
            out=ss_[b][:, :], in0=ss_[b][:, :], scalar1=1.0 / 1024.0, scalar2=EPS, op0=ALU.mult, op1=ALU.add))
        S.op("pool", [(tag + "ss", b), "nhalf"], [(tag + "rstd", b)], lambda e: e.tensor_tensor(
            out=rstd_[b][:, :], in0=ss_[b][:, :], in1=nhalf[:, 0:1], op=ALU.pow))

    def norm_n3(bufs, tag, k):
        xt_, junk_, ss_, rstd_, xn_, ptr_ = bufs
        b = k % 2
        b3 = k % len(xt_)
        S.op("dve", [(tag + "xt", b3), (tag + "rstd", b)], [(tag + "xn", b)], lambda e: e.tensor_scalar(
            out=xn_[b][:, :], in0=xt_[b3][:, :], scalar1=rstd_[b][:, 0:1], scalar2=None, op0=ALU.mult))

    def normT_pre(bufs, tag, k):
        norm_n1(bufs, tag, k)
        norm_n2(bufs, tag, k)
        norm_n3(bufs, tag, k)

    def norm_tr(bufs, tag, k):
        xt_, junk_, ss_, rstd_, xn_, ptr_ = bufs
        b = k % 2
        for c in range(8):
            S.op("pe", [(tag + "xn", b), "ident_bf"], ["p_tr"], lambda e: e.transpose(
                ptr_[:, c, :], xn_[b][:, c * 128:(c + 1) * 128], ident_bf[:, :]), last=(c == 7))

    def norm_ev(bufs, dst3, dst_res, tmp3, tmp_res="hTtmp"):
        ptr_ = bufs[5]
        S.op("dve", ["p_tr", "sc1g"], [tmp_res], lambda e: e.tensor_tensor(
            out=tmp3[:, :, :], in0=ptr_[:, :, :], in1=sc1g[:, :].unsqueeze(2).to_broadcast([128, 8, 128]), op=ALU.mult))
        S.op("pool", [tmp_res, "modT"], [dst_res], lambda e: e.tensor_tensor(
            out=dst3, in0=tmp3[:, :, :], in1=modT[:, 0:8].unsqueeze(2).to_broadcast([128, 8, 128]), op=ALU.add))

    def normT_pe(bufs, tag, k, dst_fn, dst_res):
        xt_, junk_, ss_, rstd_, xn_, ptr_ = bufs
        b = k % 2
        for c in range(8):
            S.op("pe", [(tag + "xn", b), "ident_bf"], ["p_tr"], lambda e: e.transpose(
                ptr_[:, c, :], xn_[b][:, c * 128:(c + 1) * 128], ident_bf[:, :]), last=(c == 7))
        mod_evac(ptr_, dst_fn, dst_res, sc1g, 0)

    t1b = [t1, sb(es1, "t1b", [128, 1024], BF16)]

    def P_va(gi, t, kt):
        hb, hr = hT[gi % 2], ("hT", gi % 2)
        va = va_sb[kt % 2]
        for hf in range(2):
            pp, pr = tm_proj(wA, "wA_k", OVA + hf * 512, 512, hb, hr, t)
            evac_copy(va[:, hf * 512:(hf + 1) * 512], pp[:, :], [pr], [("va", kt % 2)])

    def P_ra(gi, t, kt):
        hb, hr = hT[gi % 2], ("hT", gi % 2)
        tt = t1b[kt % 2]
        for hf in range(2):
            sl = slice(hf * 512, (hf + 1) * 512)
            pp, pr = tm_proj(wA, "wA_q", ORA + hf * 512, 512, hb, hr, t)
            S.op("act", [pr], ["thr"], lambda e: e.activation(thr[:, sl], pp[:, :], AF.Tanh, scale=0.5))
            S.op("dve", [pr, "thr"], [("t1", kt % 2)], lambda e: e.scalar_tensor_tensor(
                out=tt[:, sl], in0=thr[:, sl], scalar=1.0, in1=pp[:, :], op0=ALU.add, op1=ALU.mult))

    def P_ga(gi, t, kt):
        hb, hr = hT[gi % 2], ("hT", gi % 2)
        tt = t1b[kt % 2]
        for hf in range(2):
            sl = slice(hf * 512, (hf + 1) * 512)
            pp, pr = tm_proj(wA, "wA_q", OGA + hf * 512, 512, hb, hr, t)
            S.op("act", [pr], ["thr"], lambda e: e.activation(thr[:, sl], pp[:, :], AF.Tanh, scale=0.5))
            S.op("dve", ["thr", ("t1", kt % 2)], [("t1", kt % 2)], lambda e: e.scalar_tensor_tensor(
                out=tt[:, sl], in0=thr[:, sl], scalar=1.0, in1=tt[:, sl], op0=ALU.add, op1=ALU.mult))

    E2 = [E_sb, sb(es1, "E_sb1", [128, 4, 128])]
    Ei2 = [Einv_sb, sb(es1, "Einv_sb1", [128, 4, 128])]
    kT2 = [kinvT, sb(es1, "kinvT1", [128, 4, 128], BF16)]
    kk2 = [kinv_tok, sb(es1, "kinv_tok1", [128, 4, 128], BF16)]
    pb2 = [p_b, p_att]
    pbn = ["p_b", "p_att"]

    def G_a1(t, par=0):
        for h in range(4):
            S.op("pe", [("l_sb", t), "tri"], [pbn[par]], lambda e: e.matmul(
                pb2[par][:, h, :], l_sb[:, t, h * 128:(h + 1) * 128], tri[:, :], start=True, stop=True), last=(h == 3))

    def G_a2(par=0):
        S.op("act", [pbn[par]], [("E_sb", par)], lambda e: e.activation(E2[par][:, :, :], pb2[par][:, :, :], AF.Exp))
        S.op("act", [pbn[par]], [("Einv_sb", par)], lambda e: e.activation(
            Ei2[par][:, :, :], pb2[par][:, :, :], AF.Exp, scale=-1.0))

    def G_a3(t, is_main, par=0):
        tsl = slice(t * 128, (t + 1) * 128)
        S.op("dve", ["kaT", ("Einv_sb", par)], [("kinvT", par)], lambda e: e.tensor_tensor(
            out=kT2[par][:, :, :], in0=kaT[:, :, tsl], in1=Ei2[par][:, :, :], op=ALU.mult))
        if is_main:
            S.op("dve", ["qaT", ("E_sb", par)], ["qdecT"], lambda e: e.tensor_tensor(
                out=qdecT[:, :, :], in0=qaT[:, :, tsl], in1=E2[par][:, :, :], op=ALU.mult))

    def G_b1a(par=0):
        for h in range(4):
            S.op("pe", [("kinvT", par), "ident_bf"], ["p_tr"], lambda e: e.transpose(
                p_tr[:, h, :], kT2[par][:, h, :], ident_bf[:, :]), last=(h == 3))
        S.op("act", ["p_tr"], [("kinv_tok", par)], lambda e: e.activation(kk2[par][:, :, :], p_tr[:, 0:4, :], AF.Copy))

    def G_b1b(is_main):
        if is_main:
            for h in range(4):
                S.op("pe", [("kinvT", 0), "qdecT"], ["p_att"], lambda e: e.matmul(
                    p_att[:, h, :], kinvT[:, h, :], qdecT[:, h, :], start=True, stop=True), last=(h == 3))
            S.op("dve", ["p_att", "maskg"], ["attT_sb"], lambda e: e.tensor_tensor(
                out=attT_sb[:, :, :], in0=p_att[:, :, :], in1=maskg[:, :].unsqueeze(1).to_broadcast([128, 4, 128]),
                op=ALU.mult))

    p_T2 = [pb2[i][:, :, :].rearrange("p h q -> p (h q)").rearrange("p (j v) -> p j v", j=2) for i in range(2)]

    def G_T(kt, is_main, par=0, tb=None):
        va = va_sb[kt % 2]
        tb = par if tb is None else tb
        for h in range(4):
            dst = p_T[:, h, :] if h < 2 else p_T2[tb][:, h - 2, :]
            S.op("pe", [("kinv_tok", par), ("va", kt % 2)], ["p_T" if h < 2 else pbn[tb]], lambda e: e.matmul(
                dst, kk2[par][:, h, :], va[:, h * 256:(h + 1) * 256], start=True, stop=True), last=(h % 2 == 1))
        S.op("dve", ["p_T", "St"], ["tmpS"], lambda e: e.tensor_tensor(
            out=tmpS[:, 0:2, :], in0=p_T[:, :, :], in1=St[:, 0:2, :], op=ALU.add))
        S.op("dve", [pbn[tb], "St"], ["tmpS"], lambda e: e.tensor_tensor(
            out=tmpS[:, 2:4, :], in0=p_T2[tb], in1=St[:, 2:4, :], op=ALU.add))
        if is_main:
            S.op("dve", ["tmpS", ("E_sb", par)], ["St"], lambda e: e.tensor_tensor(
                out=St[:, :, :], in0=tmpS[:, :, :], in1=E2[par][:, :, 127:128].to_broadcast([128, 4, 256]), op=ALU.mult))
        else:
            for h in range(4):
                S.op("act", ["tmpS", ("E_sb", par)], ["St"], lambda e: e.activation(
                    St[:, h, :], tmpS[:, h, :], AF.Identity, scale=E2[par][:, h, 127:128]))
        if is_main:
            for h in range(4):
                S.op("act", ["tmpS", ("E_sb", par)], ["S_bf"], lambda e: e.activation(
                    S_bf[:, h, :], tmpS[:, h, :], AF.Identity, scale=E2[par][:, h, 127:128]))

    def G_o(kt, row0):
        va = va_sb[kt % 2]
        tt = t1b[kt % 2]
        for h in range(4):
            S.op("pe", ["attT_sb", ("va", kt % 2)], ["p_o"], lambda e: e.matmul(
                p_o[:, h * 256:(h + 1) * 256], attT_sb[:, h, :], va[:, h * 256:(h + 1) * 256],
                start=True, stop=False), last=False)
            S.op("pe", ["qdecT", "S_bf"], ["p_o"], lambda e: e.matmul(
                p_o[:, h * 256:(h + 1) * 256], qdecT[:, h, :], S_bf[:, h, :],
                start=False, stop=True), last=(h == 3))

    def G_S():
        S.op("pool", ["St"], ["S_bf"], lambda e: e.tensor_copy(S_bf[:, :, :], St[:, :, :]))

    def G_n1(kt):
        for h in range(4):
            S.op("act", ["p_o"], ["otmp", "ssq"], lambda e: e.activation(
                otmp[:, h * 256:(h + 1) * 256], p_o[:, h * 256:(h + 1) * 256], AF.Square, accum_out=ssq[:, h:h + 1]))
        S.op("dve", ["ssq"], ["ssq"], lambda e: e.tensor_scalar(
            out=ssq[:, :], in0=ssq[:, :], scalar1=4.0 / 256.0, scalar2=4.0 * EPS_GLA, op0=ALU.mult, op1=ALU.add))
        S.op("pool", ["ssq", "nhalf"], ["rso"], lambda e: e.tensor_tensor(
            out=rso[:, :], in0=ssq[:, :], in1=nhalf[:, :], op=ALU.pow))

    def G_n2(kt, row0):
        tt = t1b[kt % 2]
        ob = oag[kt % 2]
        for h in range(4):
            sl = slice(h * 256, (h + 1) * 256)
            S.op("dve", ["p_o", "rso", ("t1", kt % 2)], ["otmp"], lambda e: e.scalar_tensor_tensor(
                out=otmp[:, sl], in0=p_o[:, sl], scalar=rso[:, h:h + 1], in1=tt[:, sl], op0=ALU.mult, op1=ALU.mult))
        S.op("pool", ["otmp", "gng"], [("oag", kt % 2)], lambda e: e.tensor_tensor(
            out=ob[:, :], in0=otmp[:, :], in1=gng[:, :], op=ALU.mult))
        S.dma("sp", "oag%d" % (kt % 2), [("oag", kt % 2)], [], oa_s[row0:row0 + 128, :], ob[:, :])

    groups = [(xp, g, False) for g in range(NGP)] + [(xm, g, True) for g in range(NGM)]
    A1(groups[0][0], groups[0][1], 0)
    for c in range(8):
        S.dma("pool", "wAk", [], ["wA_k"], wA[:, c, 512:2048], w_in_r[:, c, 512:2048])
        S.dma("pool", "wAk", [], ["wA_k"], wA[:, c, 3072:3088], w_in_r[:, c, 3072:3088])
    S.dma("pool", "wgk", [], ["wgk_sb"], wgk_sb[:, :], wgk[:, :])
    S.dma("sp", "gng", [], ["gng"], gng[:, :], gng_bc[:, :])
    mod_dma(4)
    mod_dma(5)

    cast_jobs = []
    for c in range(8):
        cast_jobs.append(("wA_q", wA[:, c, 0:512], w_in_r[:, c, 0:512]))
        cast_jobs.append(("wA_q", wA[:, c, 2048:3072], w_in_r[:, c, 2048:3072]))
        cast_jobs.append(("wA_q", wA[:, c, 3088:4112], w_in_r[:, c, 4368:5392]))
    for e_ in range(16):
        cast_jobs.append((("wgu_s", e_), wgu_s[e_][:, :, 0:256], w_gate[e_].rearrange("(c p) f -> p c f", p=128)))
        cast_jobs.append((("wgu_s", e_), wgu_s[e_][:, :, 256:512], w_up[e_].rearrange("(c p) f -> p c f", p=128)))
    for c in range(8):
        for g_ in range(2):
            cast_jobs.append(("wB_s", wB_s[:, c, 0:1024].rearrange("p (m g d) -> p g m d", m=8, g=2)[:, g_],
                              w_in_r[:, c, 3088 + g_ * 512:3600 + g_ * 512].rearrange("p (m d) -> p m d", m=8)))
        cast_jobs.append(("wB_s", wB_s[:, c, 1024:1280], w_in_r[:, c, 4112:4368]))
        cast_jobs.append(("wB_s", wB_s[:, c, 1280:2304], w_in_r[:, c, 5392:6416]))
    cast_jobs.append(("wo_s", wo_s[:, :, :], w_o.rearrange("(c p) n -> p c n", p=128)))
    nslots = [len(groups) * 4]

    def cast_some():
        n = (len(cast_jobs) + nslots[0] - 1) // max(nslots[0], 1)
        nslots[0] -= 1
        for _ in range(n):
            if cast_jobs:
                res_, dst_, src_ = cast_jobs.pop(0)
                S.dma("pool", "wAq" if res_ == "wA_q" else "cast", [], [res_], dst_, src_)

    pend_n = [None]
    nxt_tiles = [(groups[gi_][0], groups[gi_][1] * 512 + t_ * 128) for gi_ in range(1, len(groups)) for t_ in range(4)]
    if nxt_tiles:
        xload(bufs1, nxt_tiles[0][0][nxt_tiles[0][1]:nxt_tiles[0][1] + 128, :], "a", kctr[0])
    for gi, (src, g, is_main) in enumerate(groups):
        if gi == NGP and NGP > 0:
            S.op("dve", ["St", "flag_sb"], ["St"], lambda e: e.tensor_scalar(
                out=St[:, :, :], in0=St[:, :, :], scalar1=flag_sb[:, 0:1], scalar2=None, op0=ALU.mult))
            S.op("pool", ["St"], ["S_bf"], lambda e: e.tensor_copy(S_bf[:, :, :], St[:, :, :]))
        B1(gi, is_main)
        kt0 = gi * 4
        P_va(gi, 0, kt0)
        if is_main:
            P_ra(gi, 0, kt0)
            P_ga(gi, 0, kt0)
        has_next = gi + 1 < len(groups)
        for t in range(4):
            kt = kt0 + t
            row0 = g * 512 + t * 128
            cast_some()
            mod_deferred()
            ka_ = None
            if has_next:
                ka_ = kctr[0]
                kctr[0] += 1
                j_ = gi * 4 + t
                if j_ + 1 < len(nxt_tiles):
                    xload(bufs1, nxt_tiles[j_ + 1][0][nxt_tiles[j_ + 1][1]:nxt_tiles[j_ + 1][1] + 128, :], "a", ka_ + 1)
            if is_main:
                G_a1(t)
                G_a2()
                G_a3(t, is_main)
                if ka_ is not None:
                    norm_n1(bufs1, "a", ka_)
                if pend_n[0] is not None:
                    G_n1(pend_n[0][0])
                if ka_ is not None:
                    norm_n2(bufs1, "a", ka_)
                if t < 3:
                    P_va(gi, t + 1, kt + 1)
                if ka_ is not None:
                    norm_n3(bufs1, "a", ka_)
                G_b1a()
                if pend_n[0] is not None:
                    G_n2(*pend_n[0])
                    pend_n[0] = None
                if t < 3:
                    P_ra(gi, t + 1, kt + 1)
                G_b1b(is_main)
                if ka_ is not None:
                    norm_tr(bufs1, "a", ka_)
                    norm_ev(bufs1, hT[(gi + 1) % 2][:, :, t * 128:(t + 1) * 128], ("hT", (gi + 1) % 2), hTtmp)
                if t < 3:
                    P_ga(gi, t + 1, kt + 1)
                G_o(kt, row0)
                pend_n[0] = (kt, row0)
                G_T(kt, is_main, 0, 1)
            else:
                par = t % 2
                if t == 0:
                    G_a1(t, par)
                    G_a2(par)
                    G_a3(t, False, par)
                    G_b1a(par)
                if ka_ is not None:
                    norm_n1(bufs1, "a", ka_)
                if t < 3:
                    P_va(gi, t + 1, kt + 1)
                    G_a1(t + 1, 1 - par)
                if ka_ is not None:
                    norm_n2(bufs1, "a", ka_)
                if t < 3:
                    G_a2(1 - par)
                if ka_ is not None:
                    norm_n3(bufs1, "a", ka_)
                G_T(kt, False, par)
                if t < 3:
                    G_a3(t + 1, False, 1 - par)
                if ka_ is not None:
                    norm_tr(bufs1, "a", ka_)
                    norm_ev(bufs1, hT[(gi + 1) % 2][:, :, t * 128:(t + 1) * 128], ("hT", (gi + 1) % 2), hTtmp)
                if t < 3:
                    G_b1a(1 - par)
    if pend_n[0] is not None:
        G_n1(pend_n[0][0])
        G_n2(*pend_n[0])
    while mod_next[0] < 12:
        mod_deferred()

    S.barrier()
    es1.close()

    es_w = ExitStack()
    wd = sb(es_w, "wd", [128, 16, 2, 1024], BF16)
    es2 = ExitStack()
    wB = sb(es2, "wB", [128, 8, 2304], BF16)
    OQB, OKB, OVB, OGB = 0, 1024, 1152, 1280
    wo = sb(es2, "wo", [128, 8, 1024], BF16)
    for c in range(8):
        S.dma("sp", "wB", ["wB_s"], ["wB"], wB[:, c, :], wB_s[:, c, :])
    S.dma("sp", "wo", ["wo_s"], ["wo"], wo[:, :, :], wo_s[:, :, :])
    wd_jobs = list(range(16))
    wd_slots = [min(NTM, 16)]

    def wd_some():
        if wd_slots[0] <= 0:
            return
        n = (len(wd_jobs) + wd_slots[0] - 1) // wd_slots[0]
        wd_slots[0] -= 1
        for _ in range(n):
            if wd_jobs:
                e_ = wd_jobs.pop(0)
                S.dma("pool", "wd", [], ["wd"], wd[:, e_, :, :], w_down[e_].rearrange("(fc p) n -> p fc n", p=128))
    p_tr = ps(es2, "p_trb", [128, 8, 128], BF16)
    p_proj = [ps(es2, "p_projb%d" % i, [128, 512]) for i in range(2)]
    p_sc = [[ps(es2, "p_sc%d%d" % (i, j), [128, 512]) for j in range(2)] for i in range(2)]
    p_ob = ps(es2, "p_ob", [128, 4, 65])
    xt = [sb(es2, "xtb%d" % i, [128, 1024]) for i in range(2)]
    junk = sb(es2, "junkb", [128, 1024], BF16)
    ss = [sb(es2, "ssb%d" % i, [128, 1]) for i in range(2)]
    rstd = [sb(es2, "rstdb%d" % i, [128, 1]) for i in range(2)]
    xn = [sb(es2, "xnb%d" % i, [128, 1024], BF16) for i in range(2)]
    hT = [sb(es2, "hTb%d" % i, [128, 8, 512], BF16) for i in range(2)]
    qbT = sb(es2, "qbT", [128, 4, 8, 128], BF16)
    kbT = sb(es2, "kbT", [128, 640], BF16)
    vba = [sb(es2, "vba%d" % i, [128, 2, 65], BF16) for i in range(3)]
    thb = sb(es2, "thb", [128, 1024], BF16)
    Pc = [sb(es2, "Pc%d" % i, [128, 4, 128], BF16) for i in range(2)]
    Pp = [sb(es2, "Pp%d" % i, [128, 4, 128], BF16) for i in range(2)]
    den = [sb(es2, "den%d" % i, [128, 4]) for i in range(2)]
    ob_sb = sb(es2, "ob_sb", [128, 16, 64])
    oa_in = [sb(es2, "oa_in%d" % i, [128, 1024], BF16) for i in range(2)]
    xr = [sb(es2, "xr%d" % i, [128, 1024]) for i in range(2)]
    merged = sb(es2, "merged", [128, 1024], BF16)
    mergedT = sb(es2, "mergedT", [128, 8, 128], BF16)
    hTtmp2 = sb(es2, "hTtmp2", [128, 8, 128])
    S.op("dve", ["wo", "gtb"], ["wo"], lambda e: e.tensor_tensor(
        out=wo[:, :, :], in0=wo[:, :, :], in1=gtb[:, 0, :].unsqueeze(1).to_broadcast([128, 8, 1024]), op=ALU.mult))
    for i in range(3):
        S.op("pool", [], [("vba", i)], lambda e: e.memset(vba[i][:, :, :], 1.0))
    S.op("pool", [], ["kbT"], lambda e: e.memset(kbT[:, :], 0.0))

    bufs2 = (xt, junk, ss, rstd, xn, p_tr)
    kctr[0] = 0
    ppq[0] = 0

    def A2(src, g, gi, tiles=(0, 1, 2, 3)):
        for t in tiles:
            r0 = g * 512 + t * 128
            ptr = normT(bufs2, src[r0:r0 + 128, :], None, sc1g, 0, "b", kctr[0])
            kctr[0] += 1
            mod_evac(ptr, lambda c: hT[gi % 2][:, c, t * 128:(t + 1) * 128], ("hT", gi % 2), sc1g, 0)

    def vb_proj(hb, hr, t, slot):
        pp, pr = tm_proj(wB, "wB", OVB, 128, hb, hr, t)
        S.op("act", [pr], [("vba", slot)], lambda e: e.activation(
            vba[slot][:, :, 0:64], pp[:, 0:128].rearrange("p (g d) -> p g d", g=2), AF.Copy))

    if NTP > 0:
        A2(xp, NGP - 1, 1, tiles=(3,))
        fm_proj(wB, "wB", OKB, 128, hT[1], ("hT", 1), 128, kbT[:, 0:128], "kbT", tok0=384)
        vb_proj(hT[1], ("hT", 1), 3, 0)

    thb2 = [thb, sb(es2, "thb2", [128, 1024], BF16)]

    def C2_loads(tg, row0):
        b2 = tg % 2
        S.dma("sp", "oain%d" % b2, [], [("oa_in", b2)], oa_in[b2][:, :], oa_s[row0:row0 + 128, :])
        S.dma("sp", "xr%d" % b2, [], [("xr", b2)], xr[b2][:, :], xm[row0:row0 + 128, :])

    def C2_P(gi, t, tg):
        hb, hr = hT[gi % 2], ("hT", gi % 2)
        vb_proj(hb, hr, t, (tg + 1) % 3)
        tb = thb2[tg % 2]
        for hf in range(2):
            sl = slice(hf * 512, (hf + 1) * 512)
            pp, pr = tm_proj(wB, "wB", OGB + hf * 512, 512, hb, hr, t)
            S.op("act", [pr], [("thb", tg % 2)], lambda e: e.activation(tb[:, sl], pp[:, :], AF.Tanh, scale=0.5))

    def C2_S(t, tg, u):
        g, j = u // 2, u % 2
        pb = u % 2
        ks = slice(g * 64, (g + 1) * 64)
        qv = qbT[ks, t, 4 * j:4 * j + 4, :]
        S.op("pe", ["kbT", "qbT"], [("p_sc", pb, 0)], lambda e: e.matmul(
            p_sc[pb][0][:, :], kbT[ks, 128 + t * 128:256 + t * 128], qv, start=True, stop=False), last=False)
        S.op("pe", ["ident_bf", "mbc"], [("p_sc", pb, 0)], lambda e: e.matmul(
            p_sc[pb][0][:, :], ident_bf[:, :], mbc[:, :, :].rearrange("p h q -> p (h q)"), start=False, stop=True))
        S.op("pe", ["kbT", "qbT"], [("p_sc", pb, 1)], lambda e: e.matmul(
            p_sc[pb][1][:, :], kbT[ks, t * 128:128 + t * 128], qv, start=True, stop=False), last=False)
        S.op("pe", ["ident_bf", "mbp"], [("p_sc", pb, 1)], lambda e: e.matmul(
            p_sc[pb][1][:, :], ident_bf[:, :], mbp[:, :, :].rearrange("p h q -> p (h q)"), start=False, stop=True))
        S.op("act", [("p_sc", pb, 0)], [("Pc", pb)], lambda e: e.activation(
            Pc[pb][:, :, :], p_sc[pb][0][:, :].rearrange("p (h q) -> p h q", h=4), AF.Exp, scale=0.125))
        if tg == 0:
            S.op("act", [("p_sc", pb, 1), "fbias"], [("Pp", pb)], lambda e: e.activation(
                Pp[pb][:, :, :], p_sc[pb][1][:, :].rearrange("p (h q) -> p h q", h=4), AF.Exp, scale=0.125,
                bias=fbias[:, 0:1]))
        else:
            S.op("act", [("p_sc", pb, 1)], [("Pp", pb)], lambda e: e.activation(
                Pp[pb][:, :, :], p_sc[pb][1][:, :].rearrange("p (h q) -> p h q", h=4), AF.Exp, scale=0.125))

    def C2_PV(tg, u):
        g, j = u // 2, u % 2
        pb = u % 2
        scur, sprev = (tg + 1) % 3, tg % 3
        for hh in range(4):
            S.op("pe", [("Pp", pb), ("vba", sprev)], ["p_ob"], lambda e: e.matmul(
                p_ob[:, hh, :], Pp[pb][:, hh, :], vba[sprev][:, g, :], start=True, stop=False), last=False)
            S.op("pe", [("Pc", pb), ("vba", scur)], ["p_ob"], lambda e: e.matmul(
                p_ob[:, hh, :], Pc[pb][:, hh, :], vba[scur][:, g, :], start=False, stop=True), last=(hh == 3))
        h0 = g * 8 + 4 * j
        S.op("dve", ["p_ob", "expsink"], [("den", pb)], lambda e: e.tensor_tensor(
            out=den[pb][:, :], in0=p_ob[:, :, 64], in1=expsink[:, h0:h0 + 4], op=ALU.add))
        S.op("dve", [("den", pb)], [("den", pb)], lambda e: e.reciprocal(den[pb][:, :], den[pb][:, :]))
        S.op("dve", ["p_ob", ("den", pb)], ["ob_sb"], lambda e: e.tensor_tensor(
            out=ob_sb[:, h0:h0 + 4, :], in0=p_ob[:, :, 0:64],
            in1=den[pb][:, :].unsqueeze(2).to_broadcast([128, 4, 64]), op=ALU.mult))

    def T_merge(tg):
        b2 = tg % 2
        tb = thb2[tg % 2]
        obf = ob_sb[:, :, :].rearrange("p h d -> p (h d)")
        S.op("dve", [("thb", tg % 2), "ob_sb"], ["ob_sb"], lambda e: e.scalar_tensor_tensor(
            out=obf, in0=tb[:, :], scalar=1.0, in1=obf, op0=ALU.add, op1=ALU.mult))
        S.op("dve", ["ob_sb", ("oa_in", b2)], ["merged"], lambda e: e.tensor_tensor(
            out=merged[:, :], in0=obf, in1=oa_in[b2][:, :], op=ALU.add))
        for c in range(8):
            S.op("pe", ["merged", "ident_bf"], ["p_tr"], lambda e: e.transpose(
                p_tr[:, c, :], merged[:, c * 128:(c + 1) * 128], ident_bf[:, :]), last=(c == 7))
        S.op("act", ["p_tr"], ["mergedT"], lambda e: e.activation(mergedT[:, :, :], p_tr[:, :, :], AF.Copy))

    def T_wo(tg, hf):
        b2 = tg % 2
        i = ppq[0] % 2
        ppq[0] += 1
        pp = p_proj[i]
        sl = slice(hf * 512, (hf + 1) * 512)
        for c in range(8):
            S.op("pe", ["mergedT", "wo"], [("pp", i)], lambda e: e.matmul(
                pp[:, :], mergedT[:, c, :], wo[:, c, sl], start=(c == 0), stop=(c == 7)), last=(c == 7))
        S.op("dve", [("pp", i), ("xr", b2)], [("xr", b2)], lambda e: e.tensor_tensor(
            out=xr[b2][:, sl], in0=pp[:, :], in1=xr[b2][:, sl], op=ALU.add))

    def T_end(tg, row0):
        b2 = tg % 2
        S.dma("sp", "x1o%d" % b2, [("xr", b2)], [], x1_s[row0:row0 + 128, :], xr[b2][:, :])

    A2(xm, 0, 0)
    prev = None
    if NGM > 1:
        xload(bufs2, xm[512:640, :], "b", kctr[0])
    C2_loads(0, 0)
    for g in range(NGM):
        hb, hr = hT[g % 2], ("hT", g % 2)
        for m in range(8):
            i = ppq[0] % 2
            ppq[0] += 1
            pp = p_proj[i]
            for c in range(8):
                S.op("pe", ["wB", hr], [("pp", i)], lambda e: e.matmul(
                    pp[:, :], wB[:, c, OQB + m * 128:OQB + (m + 1) * 128],
                    hb[:, c, :], start=(c == 0), stop=(c == 7)), last=(c == 7))
            evac_copy(qbT[:, :, m, :], pp[:, :].rearrange("p (t q) -> p t q", t=4), [("pp", i)], ["qbT"])
        fm_proj(wB, "wB", OKB, 128, hb, hr, 512, kbT[:, 128:640], "kbT")
        C2_P(g, 0, g * 4)
        for t in range(4):
            tg = g * 4 + t
            row0 = g * 512 + t * 128
            wd_some()
            ka_ = None
            if g + 1 < NGM:
                ka_ = kctr[0]
                kctr[0] += 1
                r1_ = (g + 1) * 512 + (t + 1) * 128
                if r1_ < M:
                    xload(bufs2, xm[r1_:r1_ + 128, :], "b", ka_ + 1)
                norm_n1(bufs2, "b", ka_)
            C2_S(t, tg, 0)
            C2_S(t, tg, 1)
            if ka_ is not None:
                norm_n2(bufs2, "b", ka_)
            if prev is not None:
                T_merge(prev[0])
            C2_PV(tg, 0)
            if ka_ is not None:
                norm_n3(bufs2, "b", ka_)
            C2_S(t, tg, 2)
            if prev is not None:
                T_wo(prev[0], 0)
            C2_PV(tg, 1)
            if ka_ is not None:
                norm_tr(bufs2, "b", ka_)
            C2_S(t, tg, 3)
            if ka_ is not None:
                norm_ev(bufs2, hT[(g + 1) % 2][:, :, t * 128:(t + 1) * 128], ("hT", (g + 1) % 2), hTtmp2, "hTtmp2")
            if prev is not None:
                T_wo(prev[0], 1)
                T_end(*prev)
            C2_PV(tg, 2)
            if tg + 1 < NTM:
                C2_loads(tg + 1, row0 + 128)
            C2_PV(tg, 3)
            if t < 3:
                C2_P(g, t + 1, tg + 1)
            prev = (tg, row0)
        S.op("pool", ["kbT"], ["kbT"], lambda e: e.tensor_copy(kbT[:, 0:128], kbT[:, 512:640]))
    T_merge(prev[0])
    T_wo(prev[0], 0)
    T_wo(prev[0], 1)
    T_end(*prev)

    S.barrier()
    es2.close()

    es3 = ExitStack()
    wgu = [sb(es3, "wgu%d" % i, [128, 8, 512], BF16) for i in range(3)]
    wrt = sb(es3, "wrt", [128, 8, 20]); brt = sb(es3, "brt", [1, 20]); ones1 = sb(es3, "ones1", [1, 128])
    sel = sb(es3, "sel", [16, 16, 128], BF16)
    gfb = sb(es3, "gfb", [128, 1024])
    x1t = [sb(es3, "x1c%d" % i, [128, 1024]) for i in range(2)]
    junk = sb(es3, "junkc", [128, 1024], BF16)
    ss = [sb(es3, "ssc%d" % i, [128, 1]) for i in range(2)]
    rstd = [sb(es3, "rstdc%d" % i, [128, 1]) for i in range(2)]
    xn2 = [sb(es3, "xn2_0", [128, 1024])] * 2
    h2f = [sb(es3, "h2f_0", [128, 8, 128])] * 2
    h2b = [sb(es3, "h2b%d" % i, [128, 8, 512], BF16) for i in range(2)]
    combT = [sb(es3, "combT%d" % i, [16, 512], BF16) for i in range(2)]
    cbc = [sb(es3, "cbc%d" % i, [128, 512], BF16) for i in range(2)]
    th = [sb(es3, "th%d" % i, [128, 512]) for i in range(2)]
    u1 = [sb(es3, "u1%d" % i, [128, 512]) for i in range(2)]
    hid = sb(es3, "hid", [128, 32, 512], BF16)
    xr = [sb(es3, "xrc%d" % i, [128, 1024]) for i in range(2)]
    x2 = [sb(es3, "x2%d" % i, [128, 1024]) for i in range(2)]
    sm = {n: sb(es3, "r_" + n, [128, w]) for n, w in [
        ("lg", 20), ("gmax", 1), ("gex", 4), ("gsum", 1), ("gw", 1), ("oh4", 4), ("pen", 16), ("msk", 16),
        ("m8", 8), ("oh1", 16), ("msk2", 16), ("m8b", 8), ("oh2", 16), ("dd", 1), ("w1", 1), ("w2", 1), ("comb", 16)]}
    p_tf = ps(es3, "p_tf", [128, 8, 128])
    p_small = ps(es3, "p_small", [128, 512])
    p_rt = p_small[:, 0:32]
    p_ct = p_small[0:16, 128:256]
    p_cbc = ps(es3, "p_cbc", [128, 512])
    p_gu = [ps(es3, "p_gu%d" % i, [128, 512]) for i in range(4)]

    S.dma("sp", "wrt", [], ["wrt"], wrt[:, :, :], w_rt.rearrange("(c p) n -> p c n", p=128))
    S.dma("sp", "brt", [], ["brt"], brt[:, :], b_rt[:, :])
    S.dma("sp", "gfb", [], ["gfb"], gfb[:, :], gf_bc[:, :])
    S.op("pool", [], ["ones1"], lambda e: e.memset(ones1[:, :], 1.0))
    S.op("pool", [], ["sel"], lambda e: e.memset(sel[:, :, :], 1.0))
    S.op("pool", ["sel"], ["sel"], lambda e: e.affine_select(
        out=sel[:, :, :], in_=sel[:, :, :], pattern=[[-1, 16], [0, 128]], compare_op=ALU.is_equal, fill=0.0,
        base=0, channel_multiplier=1))

    wq = [0]

    def load_expert(q):
        e_ = q % 16
        b = q % 3
        S.dma("sp", "wgu%d" % b, [], [("wgu", b)], wgu[b][:, :, :], wgu_s[e_])

    def dv(rd, wr, fn):
        S.op("dve", [("sm", r) for r in rd], [("sm", w) for w in wr], fn)

    k3 = [0]

    def A3_stages(g, gi):
        st = []
        for t in range(4):
            k = k3[0]
            k3[0] += 1
            st.extend(A3_tile(g, gi, t, k))
        return st

    def A3_tile(g, gi, t, k):
        b = k % 2
        r0 = g * 512 + t * 128
        lg = sm["lg"]

        def s1():
            if k == 0:
                S.dma("sp", "cx0", [], [("cx", 0)], x1t[0][:, :], x1_s[0:128, :])
            if (k + 1) * 128 < M:
                bn = (k + 1) % 2
                S.dma("sp", "cx%d" % bn, [], [("cx", bn)], x1t[bn][:, :], x1_s[(k + 1) * 128:(k + 2) * 128, :])
            S.op("act", [("cx", b)], ["cjunk", ("css", b)], lambda e: e.activation(
                junk[:, :], x1t[b][:, :], AF.Square, accum_out=ss[b][:, :]))
            S.op("dve", [("css", b)], [("css", b)], lambda e: e.tensor_scalar(
                out=ss[b][:, :], in0=ss[b][:, :], scalar1=1.0 / 1024.0, scalar2=EPS, op0=ALU.mult, op1=ALU.add))
            S.op("pool", [("css", b), "nhalf"], [("crs", b)], lambda e: e.tensor_tensor(
                out=rstd[b][:, :], in0=ss[b][:, :], in1=nhalf[:, 0:1], op=ALU.pow))
            S.op("dve", [("cx", b), ("crs", b)], ["xn2"], lambda e: e.tensor_scalar(
                out=xn2[b][:, :], in0=x1t[b][:, :], scalar1=rstd[b][:, 0:1], scalar2=None, op0=ALU.mult))

        def s2():
            for c in range(8):
                S.op("pe", ["xn2", "ident_f"], ["p_tf"], lambda e: e.transpose(
                    p_tf[:, c, :], xn2[b][:, c * 128:(c + 1) * 128], ident_f[:, :]), last=(c == 7))
            for c in range(8):
                S.op("dve", ["p_tf", "modT", "sc2g"], ["h2f"], lambda e: e.tensor_scalar(
                    out=h2f[b][:, c, :], in0=p_tf[:, c, :], scalar1=sc2g[:, c:c + 1], scalar2=modT[:, 24 + c:25 + c],
                    op0=ALU.mult, op1=ALU.add))
            S.op("pool", ["h2f"], [("h2b", gi % 2)], lambda e: e.tensor_copy(
                h2b[gi % 2][:, :, t * 128:(t + 1) * 128], h2f[b][:, :, :]))

        def s3():
            for c in range(8):
                S.op("pe", ["h2f", "wrt"], ["p_rt"], lambda e: e.matmul(
                    p_rt[:, 0:20], h2f[b][:, c, :], wrt[:, c, :], start=(c == 0), stop=False), last=False)
            S.op("pe", ["ones1", "brt"], ["p_rt"], lambda e: e.matmul(
                p_rt[:, 0:20], ones1[:, :], brt[:, :], start=False, stop=True))
            S.op("dve", ["p_rt"], [("sm", "lg")], lambda e: e.tensor_copy(lg[:, :], p_rt[:, 0:20]))
            dv(["lg"], ["gmax"], lambda e: e.tensor_reduce(
                out=sm["gmax"][:, :], in_=lg[:, 0:4], axis=mybir.AxisListType.X, op=ALU.max))
            dv(["lg", "gmax"], ["gex"], lambda e: e.tensor_scalar(
                out=sm["gex"][:, :], in0=lg[:, 0:4], scalar1=sm["gmax"][:, 0:1], scalar2=None, op0=ALU.subtract))
            S.op("act", [("sm", "gex")], [("sm", "gex"), ("sm", "gsum")], lambda e: e.activation(
                sm["gex"][:, :], sm["gex"][:, :], AF.Exp, accum_out=sm["gsum"][:, :]))
            dv(["gsum"], ["gw"], lambda e: e.reciprocal(sm["gw"][:, :], sm["gsum"][:, :]))
            dv(["lg", "gmax"], ["oh4"], lambda e: e.tensor_scalar(
                out=sm["oh4"][:, :], in0=lg[:, 0:4], scalar1=sm["gmax"][:, 0:1], scalar2=None, op0=ALU.is_equal))
            dv(["oh4"], ["pen"], lambda e: e.tensor_scalar(
                out=sm["pen"][:, :].rearrange("p (g j) -> p g j", g=4),
                in0=sm["oh4"][:, :].unsqueeze(2).to_broadcast([128, 4, 4]),
                scalar1=-1.0, scalar2=1.0e9, op0=ALU.add, op1=ALU.mult))
            dv(["lg", "pen"], ["msk"], lambda e: e.tensor_tensor(
                out=sm["msk"][:, :], in0=lg[:, 4:20], in1=sm["pen"][:, :], op=ALU.add))
            dv(["msk"], ["m8"], lambda e: e.max(sm["m8"][:, :], sm["msk"][:, :]))
            dv(["msk", "m8"], ["oh1"], lambda e: e.tensor_scalar(
                out=sm["oh1"][:, :], in0=sm["msk"][:, :], scalar1=sm["m8"][:, 0:1], scalar2=None, op0=ALU.is_equal))
            dv(["msk", "m8"], ["oh2"], lambda e: e.tensor_scalar(
                out=sm["oh2"][:, :], in0=sm["msk"][:, :], scalar1=sm["m8"][:, 1:2], scalar2=None, op0=ALU.is_equal))
            dv(["m8"], ["dd"], lambda e: e.tensor_tensor(
                out=sm["dd"][:, :], in0=sm["m8"][:, 1:2], in1=sm["m8"][:, 0:1], op=ALU.subtract))
            S.op("act", [("sm", "dd")], [("sm", "dd")], lambda e: e.activation(sm["dd"][:, :], sm["dd"][:, :], AF.Exp))
            dv(["dd"], ["w1"], lambda e: e.tensor_scalar(
                out=sm["w1"][:, :], in0=sm["dd"][:, :], scalar1=1.0, scalar2=None, op0=ALU.add))
            dv(["w1"], ["w1"], lambda e: e.reciprocal(sm["w1"][:, :], sm["w1"][:, :]))
            dv(["w1", "dd"], ["w2"], lambda e: e.tensor_tensor(
                out=sm["w2"][:, :], in0=sm["w1"][:, :], in1=sm["dd"][:, :], op=ALU.mult))
            dv(["w1", "gw"], ["w1"], lambda e: e.tensor_tensor(
                out=sm["w1"][:, :], in0=sm["w1"][:, :], in1=sm["gw"][:, :], op=ALU.mult))
            dv(["w2", "gw"], ["w2"], lambda e: e.tensor_tensor(
                out=sm["w2"][:, :], in0=sm["w2"][:, :], in1=sm["gw"][:, :], op=ALU.mult))
            dv(["oh1", "w1"], ["comb"], lambda e: e.tensor_scalar(
                out=sm["comb"][:, :], in0=sm["oh1"][:, :], scalar1=sm["w1"][:, 0:1], scalar2=None, op0=ALU.mult))
            dv(["oh2", "w2", "comb"], ["comb"], lambda e: e.scalar_tensor_tensor(
                out=sm["comb"][:, :], in0=sm["oh2"][:, :], scalar=sm["w2"][:, 0:1], in1=sm["comb"][:, :],
                op0=ALU.mult, op1=ALU.add))

        def s4():
            S.op("pe", [("sm", "comb"), "ident_f"], ["p_rt"], lambda e: e.transpose(
                p_ct, sm["comb"][:, :], ident_f[:, :]))
            S.op("act", ["p_rt"], [("combT", gi % 2)], lambda e: e.activation(
                combT[gi % 2][:, t * 128:(t + 1) * 128], p_ct, AF.Copy))
        return [s1, s2, s3, s4]

    NG = NGM
    load_expert(0)
    load_expert(1)
    load_expert(2)
    for f in A3_stages(0, 0):
        f()
    for g in range(NG):
        hb, hr = h2b[g % 2], ("h2b", g % 2)
        nxt = A3_stages(g + 1, g + 1) if g + 1 < NG else []
        for e_ in range(16):
            q = g * 16 + e_
            b = q % 3
            cb = q % 2
            S.op("pe", ["sel", ("combT", g % 2)], ["p_cbc"], lambda e: e.matmul(
                p_cbc[:, :], sel[:, e_, :], combT[g % 2][:, :], start=True, stop=True))
            S.op("act", ["p_cbc"], [("cbc", cb)], lambda e: e.activation(cbc[cb][:, :], p_cbc[:, :], AF.Copy))
            for fc in range(2):
                pg, pu = p_gu[fc * 2], p_gu[fc * 2 + 1]
                rg, ru = ("p_gu", fc * 2), ("p_gu", fc * 2 + 1)
                for c in range(8):
                    S.op("pe", [("wgu", b), hr], [rg], lambda e: e.matmul(
                        pg[:, :], wgu[b][:, c, fc * 128:(fc + 1) * 128], hb[:, c, :],
                        start=(c == 0), stop=(c == 7)), last=(c == 7))
                for c in range(8):
                    S.op("pe", [("wgu", b), hr], [ru], lambda e: e.matmul(
                        pu[:, :], wgu[b][:, c, 256 + fc * 128:256 + (fc + 1) * 128], hb[:, c, :],
                        start=(c == 0), stop=(c == 7)), last=(c == 7))
                S.op("act", [rg], [("th", fc)], lambda e: e.activation(th[fc][:, :], pg[:, :], AF.Tanh, scale=0.5))
                S.op("dve", [rg, ("th", fc)], [("th", fc)], lambda e: e.scalar_tensor_tensor(
                    out=th[fc][:, :], in0=th[fc][:, :], scalar=1.0, in1=pg[:, :], op0=ALU.add, op1=ALU.mult))
                S.op("dve", [ru, ("th", fc)], [("u1", fc)], lambda e: e.tensor_tensor(
                    out=u1[fc][:, :], in0=th[fc][:, :], in1=pu[:, :], op=ALU.mult))
                S.op("pool", [("u1", fc), ("cbc", cb)], [("hid", e_ * 2 + fc)], lambda e: e.tensor_tensor(
                    out=hid[:, e_ * 2 + fc, :], in0=u1[fc][:, :], in1=cbc[cb][:, :], op=ALU.mult))
            if q + 3 < NG * 16:
                load_expert(q + 3)
            if e_ < len(nxt):
                nxt[e_]()
        for t in range(4):
            k = g * 4 + t
            b = k % 2
            r0 = g * 512 + t * 128
            S.dma("sp", "xrc%d" % b, [], [("xrc", b)], xr[b][:, :], x1_s[r0:r0 + 128, :])
            for hf in range(2):
                pi = (k * 2 + hf) % 4
                py = p_gu[pi]
                sl = slice(hf * 512, (hf + 1) * 512)
                for ef in range(32):
                    S.op("pe", [("hid", ef), "wd"], [("p_gu", pi)], lambda e: e.matmul(
                        py[:, :], hid[:, ef, t * 128:(t + 1) * 128], wd[:, ef // 2, ef % 2, sl],
                        start=(ef == 0), stop=(ef == 31)), last=(ef == 31))
                S.op("dve", [("p_gu", pi), "gtb"], [("x2", b)], lambda e: e.tensor_tensor(
                    out=x2[b][:, sl], in0=py[:, :], in1=gtb[:, 1, sl], op=ALU.mult))
                S.op("pool", [("x2", b), ("xrc", b)], [("x2", b)], lambda e: e.tensor_tensor(
                    out=x2[b][:, sl], in0=x2[b][:, sl], in1=xr[b][:, sl], op=ALU.add))
            S.op("act", [("x2", b)], ["cjunk", ("fss", b)], lambda e: e.activation(
                junk[:, :], x2[b][:, :], AF.Square, accum_out=ss[b][:, :]))
            S.op("dve", [("fss", b)], [("fss", b)], lambda e: e.tensor_scalar(
                out=ss[b][:, :], in0=ss[b][:, :], scalar1=1.0 / 1024.0, scalar2=EPS, op0=ALU.mult, op1=ALU.add))
            S.op("pool", [("fss", b), "nhalf"], [("frs", b)], lambda e: e.tensor_tensor(
                out=rstd[b][:, :], in0=ss[b][:, :], in1=nhalf[:, 0:1], op=ALU.pow))
            S.op("dve", [("x2", b), ("frs", b), "gfb"], [("x2", b)], lambda e: e.scalar_tensor_tensor(
                out=x2[b][:, :], in0=x2[b][:, :], scalar=rstd[b][:, 0:1], in1=gfb[:, :], op0=ALU.mult, op1=ALU.mult))
            S.dma("sp", "out%d" % b, [("x2", b)], [], out[r0:r0 + 128, :], x2[b][:, :])

    S.finish("sp")
    es3.close()
    es_w.close()
    es0.close()
    return nc


def make_in_maps(inputs, n_cores, NTM, NTP, seq):
    f = lambda a: np.ascontiguousarray(np.asarray(a, dtype=np.float32))
    x = f(inputs["x"]); c = f(inputs["c"])
    w_ada = f(inputs["w_ada"][0]); b_ada = f(inputs["b_ada"][0])
    pm = lambda v: np.ascontiguousarray(v.reshape(-1, 128).T)
    rep = lambda v: np.ascontiguousarray(np.broadcast_to(v[None, :], (128, v.shape[0])))
    shared = {
        "w_ada": w_ada, "bada_pm": pm(b_ada),
        "bada_bc": np.ascontiguousarray(np.stack([rep(b_ada[2048:3072]), rep(b_ada[5120:6144])], axis=1)),
        "g1_pm": pm(f(inputs["norm1_g"][0])), "g2_pm": pm(f(inputs["norm2_g"][0])),
        "gf_bc": rep(f(inputs["norm_f_g"])),
        "w_in": f(inputs["w_in"][0]),
        "wgk": np.ascontiguousarray(np.concatenate([f(inputs["w_gk2"][0]), f(inputs["b_gk"][0])[None, :]], axis=0)),
        "gng_bc": rep(np.tile(f(inputs["gla_norm_g"][0]), 4)),
        "sink_bc": rep(f(inputs["sink"][0])),
        "w_o": f(inputs["w_o"][0]),
        "w_rt": np.ascontiguousarray(np.concatenate([f(inputs["w_group"][0]), f(inputs["w_router"][0])], axis=1)),
        "b_rt": np.ascontiguousarray(np.concatenate([f(inputs["b_group"][0]), f(inputs["b_router"][0])])[None, :]),
        "w_gate": f(inputs["w_gate"][0]), "w_up": f(inputs["w_up"][0]), "w_down": f(inputs["w_down"][0]),
    }
    M, P = NTM * 128, NTP * 128
    maps = []
    for k in range(n_cores):
        b, half = k // 2, k % 2
        m = dict(shared)
        m["xm"] = np.ascontiguousarray(x[b, half * M:(half + 1) * M])
        if half == 1:
            m["xp"] = np.ascontiguousarray(x[b, 0:P])
        else:
            m["xp"] = np.zeros((P, 1024), np.float32)
        m["flag"] = np.full((128, 1), float(half), np.float32)
        m["c_pm"] = pm(c[b])
        maps.append(m)
    return maps


def kernel(**inputs):
    NTM = NTP = 32
    nc = bass.Bass("TRN2", target_bir_lowering=False)
    build(nc, NTM, NTP)
    maps = make_in_maps(inputs, 8, NTM, NTP, 8192)
    res = run_bass_kernel_spmd(nc, maps, core_ids=list(range(8)))
    out = np.zeros((4, 8192, 1024), np.float32)
    for k in range(8):
        b, half = k // 2, k % 2
        out[b, half * 4096:(half + 1) * 4096] = res.results[k]["out"]
    return out
```
